# Optimizing a Trainium2 kernel written in Bass

```python
import math
import jax
import jax.numpy as jnp
from jax import lax
import numpy as np

D_MODEL = 1024
BATCH = 4
SEQ = 4096
DEPTH = 1

MEM_LEN = 256
EPS = 1e-6

SSD_EXPAND = 2
SSD_D_INNER = SSD_EXPAND * D_MODEL
SSD_HEAD_DIM = 64
SSD_HEADS = SSD_D_INNER // SSD_HEAD_DIM
SSD_GROUPS = 8
SSD_HEADS_PER_GROUP = SSD_HEADS // SSD_GROUPS
SSD_STATE = 128
SSD_CONV = 4
SSD_CHUNK = 128
SSD_CONV_DIM = SSD_D_INNER + 2 * SSD_GROUPS * SSD_STATE

DIL_PATTERNS = ((128, 1), (512, 4), (2048, 16))
DIL_GROUPS = len(DIL_PATTERNS)
DIL_HEADS_PER_GROUP = 8
DIL_HEAD_DIM = 64
DIL_WIDTH = DIL_GROUPS * DIL_HEADS_PER_GROUP * DIL_HEAD_DIM
DIL_OUT_WIDTH = DIL_HEADS_PER_GROUP * DIL_HEAD_DIM
DIL_BLOCK = 128
ROPE_THETA = 10000.0

MEM_HEADS = 4
MEM_HEAD_DIM = 384
MEM_WIDTH = MEM_HEADS * MEM_HEAD_DIM

N_BRANCHES = 3
IN_WIDTHS = (SSD_D_INNER, SSD_CONV_DIM, SSD_HEADS, DIL_WIDTH, DIL_WIDTH, DIL_WIDTH, MEM_WIDTH, N_BRANCHES * D_MODEL)
D_IN_PROJ = sum(IN_WIDTHS)

PEER_N_KEYS = 128
PEER_N_EXPERTS = PEER_N_KEYS * PEER_N_KEYS
PEER_HEADS = 8
PEER_TOPK = 16
PEER_QUERY_DIM = 256
PEER_HALF = PEER_QUERY_DIM // 2
PEER_BLOCK = 128

kernel_name = 'hybrid_ssd_dilated_memory_peer_block'


def _offsets(widths):
    out, acc = [], 0
    for w in widths[:-1]:
        acc += w
        out.append(acc)
    return out


def rmsnorm(x, gain):
    xf = x.astype(jnp.float32)
    y = xf * lax.rsqrt(jnp.mean(xf * xf, axis=-1, keepdims=True) + EPS)
    return (y * gain.astype(jnp.float32)).astype(x.dtype)


def rope(t, positions):
    half = t.shape[-1] // 2
    inv_freq = ROPE_THETA ** (-jnp.arange(half, dtype=jnp.float32) / half)
    ang = positions.astype(jnp.float32)[:, :, None, None] * inv_freq
    cos, sin = jnp.cos(ang), jnp.sin(ang)
    tf = t.astype(jnp.float32)
    t1, t2 = tf[..., :half], tf[..., half:]
    return jnp.concatenate([t1 * cos - t2 * sin, t2 * cos + t1 * sin], axis=-1).astype(t.dtype)


def causal_depthwise_conv(x, w, b):
    k_width, channels = w.shape
    y = lax.conv_general_dilated(x, w[:, None, :].astype(x.dtype), window_strides=(1,),
                                 padding=[(k_width - 1, 0)],
                                 dimension_numbers=('NWC', 'WIO', 'NWC'),
                                 feature_group_count=channels)
    return y + b.astype(x.dtype)


def segsum(a):
    t = a.shape[-1]
    cs = jnp.cumsum(a, axis=-1)
    seg = cs[..., :, None] - cs[..., None, :]
    return jnp.where(jnp.tril(jnp.ones((t, t), dtype=bool)), seg, -jnp.inf)


def ssd_chunked_scan(x, a, b, c):
    bsz, seq, g, r, p = x.shape
    n = b.shape[-1]
    nc = seq // SSD_CHUNK
    x = x.reshape(bsz, nc, SSD_CHUNK, g, r, p)
    b = b.reshape(bsz, nc, SSD_CHUNK, g, n)
    c = c.reshape(bsz, nc, SSD_CHUNK, g, n)
    a = a.reshape(bsz, nc, SSD_CHUNK, g, r).transpose(0, 3, 4, 1, 2)
    a_cs = jnp.cumsum(a, axis=-1)
    decay = jnp.exp(segsum(a)).astype(x.dtype)
    cb = jnp.einsum('bclgn,bcsgn->bcgls', c, b)
    y_diag = jnp.einsum('bcgls,bgrcls,bcsgrp->bclgrp', cb, decay, x)
    decay_states = jnp.exp(a_cs[..., -1:] - a_cs).astype(x.dtype)
    states = jnp.einsum('bclgn,bgrcl,bclgrp->bcgrpn', b, decay_states, x)
    states = jnp.concatenate([jnp.zeros_like(states[:, :1]), states], axis=1)
    chunk_ends = jnp.pad(a_cs[..., -1], ((0, 0), (0, 0), (0, 0), (1, 0)))
    chunk_decay = jnp.exp(segsum(chunk_ends)).astype(x.dtype)
    states = jnp.einsum('bgrzc,bcgrpn->bzgrpn', chunk_decay, states)[:, :-1]
    y_off = jnp.einsum('bclgn,bcgrpn,bgrcl->bclgrp', c, states, jnp.exp(a_cs).astype(x.dtype))
    return (y_diag + y_off).reshape(bsz, seq, g, r, p)


def ssd_mixer(z, xbc, dt, conv_w, conv_b, dt_bias, a_log, d_skip, ssd_norm):
    bsz, seq, _ = z.shape
    g, r, p, n = SSD_GROUPS, SSD_HEADS_PER_GROUP, SSD_HEAD_DIM, SSD_STATE
    xbc = jax.nn.silu(causal_depthwise_conv(xbc, conv_w, conv_b))
    xs, bs, cs = jnp.split(xbc, [SSD_D_INNER, SSD_D_INNER + g * n], axis=-1)
    xs = xs.reshape(bsz, seq, g, r, p)
    bs = bs.reshape(bsz, seq, g, n)
    cs = cs.reshape(bsz, seq, g, n)
    dt = jax.nn.softplus(dt.astype(jnp.float32) + dt_bias.astype(jnp.float32)).reshape(bsz, seq, g, r)
    a = -jnp.exp(a_log.astype(jnp.float32)).reshape(g, r)
    y = ssd_chunked_scan(xs * dt[..., None].astype(xs.dtype), a * dt, bs, cs)
    y = y + d_skip.reshape(g, r)[:, :, None].astype(xs.dtype) * xs
    y = y.reshape(bsz, seq, SSD_D_INNER)
    return rmsnorm(y * jax.nn.silu(z), ssd_norm)


def dilated_group(q, k, v, window, dilation):
    bsz, seq, h, dh = q.shape
    steps = window // dilation
    s_len = seq // dilation
    nb = -(-s_len // DIL_BLOCK)
    s_pad = nb * DIL_BLOCK

    def to_stream(t):
        return t.reshape(bsz, s_len, dilation, h, dh).transpose(0, 2, 3, 1, 4)

    qs, ks, vs = to_stream(q), to_stream(k), to_stream(v)
    qs = jnp.pad(qs, ((0, 0), (0, 0), (0, 0), (0, s_pad - s_len), (0, 0))).reshape(bsz, dilation, h, nb, DIL_BLOCK, dh)
    kv_pad = ((0, 0), (0, 0), (0, 0), (DIL_BLOCK, s_pad - s_len), (0, 0))

    def band(t):
        t = jnp.pad(t, kv_pad)
        prev = t[:, :, :, :s_pad].reshape(bsz, dilation, h, nb, DIL_BLOCK, dh)
        cur = t[:, :, :, DIL_BLOCK:].reshape(bsz, dilation, h, nb, DIL_BLOCK, dh)
        return jnp.concatenate([prev, cur], axis=4)

    kb, vb = band(ks), band(vs)
    s = jnp.einsum('brhnqd,brhnkd->brhnqk', qs, kb, preferred_element_type=jnp.float32) / math.sqrt(dh)
    qi = jnp.arange(nb)[:, None] * DIL_BLOCK + jnp.arange(DIL_BLOCK)[None, :]
    kj = jnp.arange(nb)[:, None] * DIL_BLOCK - DIL_BLOCK + jnp.arange(2 * DIL_BLOCK)[None, :]
    dist = qi[:, :, None] - kj[:, None, :]
    mask = (dist >= 0) & (dist <= steps) & (kj[:, None, :] >= 0)
    s = jnp.where(mask, s, -jnp.inf)
    m = jnp.max(s, axis=-1, keepdims=True)
    pexp = jnp.exp(s - m)
    den = jnp.sum(pexp, axis=-1, keepdims=True)
    o = jnp.einsum('brhnqk,brhnkd->brhnqd', (pexp / den).astype(v.dtype), vb)
    lse = (m + jnp.log(den))[..., 0]
    o = o.reshape(bsz, dilation, h, s_pad, dh)[:, :, :, :s_len].transpose(0, 3, 1, 2, 4).reshape(bsz, seq, h, dh)
    lse = lse.reshape(bsz, dilation, h, s_pad)[..., :s_len].transpose(0, 3, 1, 2).reshape(bsz, seq, h)
    return o, lse


def dilated_attention(q, k, v, positions, q_gain, k_gain):
    bsz, seq, _ = q.shape
    h_all = DIL_GROUPS * DIL_HEADS_PER_GROUP
    q = rope(rmsnorm(q.reshape(bsz, seq, h_all, DIL_HEAD_DIM), q_gain), positions)
    k = rope(rmsnorm(k.reshape(bsz, seq, h_all, DIL_HEAD_DIM), k_gain), positions)
    v = v.reshape(bsz, seq, h_all, DIL_HEAD_DIM)
    outs, lses = [], []
    for g, (window, dilation) in enumerate(DIL_PATTERNS):
        sl = slice(g * DIL_HEADS_PER_GROUP, (g + 1) * DIL_HEADS_PER_GROUP)
        o, lse = dilated_group(q[:, :, sl], k[:, :, sl], v[:, :, sl], window, dilation)
        outs.append(o)
        lses.append(lse)
    o = jnp.stack(outs, axis=2)
    w = jax.nn.softmax(jnp.stack(lses, axis=2), axis=2)
    return jnp.einsum('blgh,blghd->blhd', w.astype(o.dtype), o).reshape(bsz, seq, DIL_OUT_WIDTH)


def memory_attention(q, mem, mem_norm, w_mem_kv, q_gain, k_gain):
    bsz, seq, _ = q.shape
    kv = jnp.einsum('bmd,de->bme', rmsnorm(mem, mem_norm), w_mem_kv)
    k, v = jnp.split(kv, 2, axis=-1)
    q = rmsnorm(q.reshape(bsz, seq, MEM_HEADS, MEM_HEAD_DIM), q_gain)
    k = rmsnorm(k.reshape(bsz, -1, MEM_HEADS, MEM_HEAD_DIM), k_gain)
    v = v.reshape(bsz, -1, MEM_HEADS, MEM_HEAD_DIM)
    s = jnp.einsum('blhd,bmhd->bhlm', q, k, preferred_element_type=jnp.float32) / math.sqrt(MEM_HEAD_DIM)
    p = jax.nn.softmax(s, axis=-1).astype(v.dtype)
    return jnp.einsum('bhlm,bmhd->blhd', p, v).reshape(bsz, seq, MEM_WIDTH)


def mixer_sublayer(x, mem, positions, norm_mix, w_in, conv_w, conv_b, dt_bias, a_log, d_skip, ssd_norm,
                   dil_q_norm, dil_k_norm, mem_norm, w_mem_kv, mem_q_norm, mem_k_norm,
                   w_up_ssd, w_up_dil, w_up_mem, w_out):
    bsz, seq, _ = x.shape
    h = rmsnorm(x, norm_mix)
    proj = jnp.einsum('bld,de->ble', h, w_in)
    z, xbc, dt, q_d, k_d, v_d, q_m, gate_logits = jnp.split(proj, _offsets(IN_WIDTHS), axis=-1)
    y_ssd = ssd_mixer(z, xbc, dt, conv_w, conv_b, dt_bias, a_log, d_skip, ssd_norm)
    y_dil = dilated_attention(q_d, k_d, v_d, positions, dil_q_norm, dil_k_norm)
    y_mem = memory_attention(q_m, mem, mem_norm, w_mem_kv, mem_q_norm, mem_k_norm)
    gates = jax.nn.sigmoid(gate_logits.astype(jnp.float32)).astype(x.dtype).reshape(bsz, seq, N_BRANCHES, D_MODEL)
    merged = (gates[:, :, 0] * (y_ssd @ w_up_ssd)
              + gates[:, :, 1] * (y_dil @ w_up_dil)
              + gates[:, :, 2] * (y_mem @ w_up_mem))
    return merged @ w_out


def peer_ffn(h, w_q, keys1, keys2, u_table, v_table):
    bsz, seq, d = h.shape
    t = bsz * seq
    ht = h.reshape(t, d)
    q = (ht @ w_q).reshape(t, PEER_HEADS, 2, PEER_HALF)
    s1 = jnp.einsum('thd,kd->thk', q[:, :, 0], keys1, preferred_element_type=jnp.float32)
    s2 = jnp.einsum('thd,kd->thk', q[:, :, 1], keys2, preferred_element_type=jnp.float32)
    v1, i1 = lax.top_k(s1, PEER_TOPK)
    v2, i2 = lax.top_k(s2, PEER_TOPK)
    cand = (v1[..., :, None] + v2[..., None, :]).reshape(t, PEER_HEADS, PEER_TOPK * PEER_TOPK)
    sc, ci = lax.top_k(cand, PEER_TOPK)
    e1 = jnp.take_along_axis(i1, ci // PEER_TOPK, axis=-1)
    e2 = jnp.take_along_axis(i2, ci % PEER_TOPK, axis=-1)
    expert = e1 * PEER_N_KEYS + e2
    gate = jax.nn.softmax(sc, axis=-1)
    nblk = t // PEER_BLOCK

    def block(args):
        hb, eb, gb = args
        act = jax.nn.gelu(jnp.einsum('thkd,td->thk', u_table[eb], hb), approximate=False)
        return jnp.einsum('thk,thkd->td', gb.astype(hb.dtype) * act, v_table[eb])

    out = lax.map(block, (ht.reshape(nblk, PEER_BLOCK, d),
                          expert.reshape(nblk, PEER_BLOCK, PEER_HEADS, PEER_TOPK),
                          gate.reshape(nblk, PEER_BLOCK, PEER_HEADS, PEER_TOPK)))
    return out.reshape(bsz, seq, d)


def setup_inputs(seed: int = 0) -> dict:
    key = jax.random.key(seed)
    ks = jax.random.split(key, 32)
    f32 = jnp.float32

    def nrm(k, shape, scale):
        return jax.random.normal(k, shape, f32) * scale

    def gain(k, n):
        return 1.0 + 0.02 * jax.random.normal(k, (DEPTH, n), f32)

    x = nrm(ks[0], (BATCH, SEQ, D_MODEL), 1.0)
    mem = nrm(ks[1], (BATCH, MEM_LEN, D_MODEL), 1.0)
    positions = (jax.random.randint(ks[2], (BATCH, 1), 0, 1024, dtype=jnp.int32)
                 + jnp.arange(SEQ, dtype=jnp.int32)[None, :])
    dt_init = jnp.exp(jax.random.uniform(ks[6], (DEPTH, SSD_HEADS), f32, math.log(1e-3), math.log(1e-1)))
    dt_bias = dt_init + jnp.log(-jnp.expm1(-dt_init))
    a_log = jnp.log(jax.random.uniform(ks[7], (DEPTH, SSD_HEADS), f32, 1.0, 16.0))
    return {
        'x': x,
        'mem': mem,
        'positions': positions,
        'norm_mix': gain(ks[3], D_MODEL),
        'w_in': nrm(ks[4], (DEPTH, D_MODEL, D_IN_PROJ), D_MODEL ** -0.5),
        'conv_w': nrm(ks[5], (DEPTH, SSD_CONV, SSD_CONV_DIM), SSD_CONV ** -0.5),
        'conv_b': nrm(ks[8], (DEPTH, SSD_CONV_DIM), 0.02),
        'dt_bias': dt_bias,
        'a_log': a_log,
        'd_skip': 1.0 + 0.02 * jax.random.normal(ks[9], (DEPTH, SSD_HEADS), f32),
        'ssd_norm': gain(ks[10], SSD_D_INNER),
        'dil_q_norm': gain(ks[11], DIL_HEAD_DIM),
        'dil_k_norm': gain(ks[12], DIL_HEAD_DIM),
        'mem_norm': gain(ks[13], D_MODEL),
        'w_mem_kv': nrm(ks[14], (DEPTH, D_MODEL, 2 * MEM_WIDTH), D_MODEL ** -0.5),
        'mem_q_norm': gain(ks[15], MEM_HEAD_DIM),
        'mem_k_norm': gain(ks[16], MEM_HEAD_DIM),
        'w_up_ssd': nrm(ks[17], (DEPTH, SSD_D_INNER, D_MODEL), SSD_D_INNER ** -0.5),
        'w_up_dil': nrm(ks[18], (DEPTH, DIL_OUT_WIDTH, D_MODEL), DIL_OUT_WIDTH ** -0.5),
        'w_up_mem': nrm(ks[19], (DEPTH, MEM_WIDTH, D_MODEL), MEM_WIDTH ** -0.5),
        'w_out': nrm(ks[20], (DEPTH, D_MODEL, D_MODEL), D_MODEL ** -0.5),
        'norm_ffn': gain(ks[21], D_MODEL),
        'peer_w_q': nrm(ks[22], (DEPTH, D_MODEL, PEER_HEADS * PEER_QUERY_DIM), D_MODEL ** -0.5),
        'peer_keys1': nrm(ks[23], (DEPTH, PEER_N_KEYS, PEER_HALF), PEER_HALF ** -0.5),
        'peer_keys2': nrm(ks[24], (DEPTH, PEER_N_KEYS, PEER_HALF), PEER_HALF ** -0.5),
        'peer_u': nrm(ks[25], (DEPTH, PEER_N_EXPERTS, D_MODEL), D_MODEL ** -0.5),
        'peer_v': nrm(ks[26], (DEPTH, PEER_N_EXPERTS, D_MODEL), PEER_HEADS ** -0.5),
    }


def reference(x, mem, positions, norm_mix, w_in, conv_w, conv_b, dt_bias, a_log, d_skip, ssd_norm,
              dil_q_norm, dil_k_norm, mem_norm, w_mem_kv, mem_q_norm, mem_k_norm,
              w_up_ssd, w_up_dil, w_up_mem, w_out, norm_ffn, peer_w_q, peer_keys1, peer_keys2,
              peer_u, peer_v):
    for layer in range(DEPTH):
        x = x + mixer_sublayer(x, mem, positions, norm_mix[layer], w_in[layer], conv_w[layer], conv_b[layer],
                               dt_bias[layer], a_log[layer], d_skip[layer], ssd_norm[layer],
                               dil_q_norm[layer], dil_k_norm[layer], mem_norm[layer], w_mem_kv[layer],
                               mem_q_norm[layer], mem_k_norm[layer], w_up_ssd[layer], w_up_dil[layer],
                               w_up_mem[layer], w_out[layer])
        x = x + peer_ffn(rmsnorm(x, norm_ffn[layer]), peer_w_q[layer], peer_keys1[layer], peer_keys2[layer],
                         peer_u[layer], peer_v[layer])
    return x
```

```python
import math
import types
from contextlib import ExitStack

import numpy as np
import concourse.bass as bass
import concourse.mybir as mybir
from concourse.bass_utils import run_bass_kernel_spmd

F32 = mybir.dt.float32
BF16 = mybir.dt.bfloat16
I32 = mybir.dt.int32
U32 = mybir.dt.uint32
AF = mybir.ActivationFunctionType
ALU = mybir.AluOpType
AX = mybir.AxisListType

EPS = 1e-6
DIL_STOP = 99
NT = 16
TOK = 2048
C_Z, C_X, C_B, C_C, C_DT, C_QD, C_KD, C_VD, C_QM, C_G = 0, 2048, 4096, 5120, 6144, 6176, 7712, 9248, 10784, 12320
K_ID, K_TRIU, K_TRIL, K_TRILS, K_ONES, K_BM, K_RM, K_SH0, K_SH1, K_LO, K_INVF, K_IOTA, K_INVFC, K_END = 0, 128, 256, 384, 512, 640, 768, 896, 1024, 1152, 1280, 1312, 1328, 1332


def _freeze(fn):
    if fn.__closure__ is None:
        return fn
    cells = []
    for c in fn.__closure__:
        try:
            cells.append(types.CellType(c.cell_contents))
        except ValueError:
            cells.append(c)
    return types.FunctionType(fn.__code__, fn.__globals__, fn.__name__, fn.__defaults__, tuple(cells))


class Buf:
    __slots__ = ('name', 'w', 'r', 'sem', 'semcnt')

    def __init__(self, name):
        self.name = name
        self.w = {}
        self.r = {}
        self.sem = None
        self.semcnt = 0


class Sched:
    ENG = ('pe', 'act', 'dve', 'pool', 'sp')

    def __init__(self, nc):
        self.nc = nc
        self.prog = {e: [] for e in self.ENG}
        self.cnt = {e: 0 for e in self.ENG}
        self.sem = {e: nc.alloc_semaphore('prog_' + e) for e in self.ENG}
        self.seen = {e: {} for e in self.ENG}
        self.dmabufs = []
        self.nwait = 0

    def _waits(self, e, toks):
        need = {}
        for key, (sem, val, src) in toks:
            if src == 'pe' and e == 'pe':
                continue
            if self.seen[e].get(key, 0) >= val:
                continue
            if key not in need or need[key][1] < val:
                need[key] = (sem, val)
        for key, (sem, val) in need.items():
            self.seen[e][key] = val
            self.prog[e].append(('wait', sem, val))
            self.nwait += 1

    @staticmethod
    def _merge(d, key, tok):
        if key not in d or d[key][1] < tok[1]:
            d[key] = tok

    @staticmethod
    def _deps(reads, writes):
        toks = []
        for b in reads:
            toks += list(b.w.items())
        for b in writes:
            toks += list(b.w.items())
            toks += list(b.r.items())
        return toks

    def op(self, e, fn, reads=(), writes=()):
        self._waits(e, self._deps(reads, writes))
        self.cnt[e] += 1
        tok = (self.sem[e], self.cnt[e], e)
        self.prog[e].append(('op', _freeze(fn), self.sem[e], 1))
        for b in reads:
            self._merge(b.r, e, tok)
        for b in writes:
            b.w = {e: tok}
            b.r = {}

    def dma(self, q, out_ap, in_ap, reads, writes, sembuf, indirect=None, **kw):
        if sembuf.sem is None:
            sembuf.sem = self.nc.alloc_semaphore('dma_' + sembuf.name)
            self.dmabufs.append(sembuf)
        key = 'dma_' + sembuf.name
        toks = self._deps(reads, writes)
        if sembuf.semcnt:
            toks.append((key, (sembuf.sem, sembuf.semcnt, None)))
        self._waits(q, toks)
        sembuf.semcnt += 16
        tok = (sembuf.sem, sembuf.semcnt, None)
        if indirect is None:
            fn = lambda e: e.dma_start(out=out_ap, in_=in_ap, **kw)
        else:
            fn = _freeze(indirect)
        self.prog[q].append(('op', fn, sembuf.sem, 16))
        for b in reads:
            self._merge(b.r, key, tok)
        for b in writes:
            b.w = {key: tok}
            b.r = {}

    def barrier(self):
        toks = []
        for e in self.ENG:
            if self.cnt[e]:
                toks.append((e, (self.sem[e], self.cnt[e], None)))
        for b in self.dmabufs:
            toks.append(('dma_' + b.name, (b.sem, b.semcnt, None)))
        for e in self.ENG:
            self._waits(e, toks)

    def release_dma_sems(self, bufs):
        pass

    def finish(self):
        self.barrier()
        nc = self.nc
        prog = self.prog

        def mk(e):
            def body(engine):
                for item in prog[e]:
                    if item[0] == 'wait':
                        engine.wait_ge(item[1], item[2])
                    else:
                        item[1](engine).then_inc(item[2], item[3])
            return body
        with nc.Block() as block:
            block.sync(mk('sp'))
            block.scalar(mk('act'))
            block.vector(mk('dve'))
            block.gpsimd(mk('pool'))
            block.tensor(mk('pe'))


def build(upto=99, dbg=()):
    nc = bass.Bass("TRN2", target_bir_lowering=False)
    S = Sched(nc)
    op = S.op

    def din(name, shape, dt=F32):
        return nc.dram_tensor(name, shape, dt, kind="ExternalInput").ap()

    xa = din('xa', [4096, 1024])
    posa = din('posa', [4096], I32)
    flag_d = din('flag', [128, 1])
    mem_d = din('mem', [256, 1024])
    cst_d = din('cst', [128, K_END])
    norm_mix = din('norm_mix', [1, 1024])
    w_in = din('w_in', [1024, 15392])
    conv_w = din('conv_w', [4, 4096])
    conv_b = din('conv_b', [1, 4096])
    dt_bias = din('dt_bias', [1, 32])
    a_log = din('a_log', [1, 32])
    d_skip = din('d_skip', [1, 32])
    ssd_norm = din('ssd_norm', [1, 2048])
    dil_q_norm = din('dil_q_norm', [1, 64])
    dil_k_norm = din('dil_k_norm', [1, 64])
    gq_col = din('gq_col', [128, 1])
    gk_col = din('gk_col', [128, 1])
    mem_norm = din('mem_norm', [1, 1024])
    w_mem_kv = din('w_mem_kv', [1024, 3072])
    mem_q_norm = din('mem_q_norm', [1, 384])
    mem_k_norm = din('mem_k_norm', [1, 384])
    w_up_ssd = din('w_up_ssd', [2048, 1024])
    w_up_dil = din('w_up_dil', [512, 1024])
    w_up_mem = din('w_up_mem', [1536, 1024])
    w_out = din('w_out', [1024, 1024])
    norm_ffn = din('norm_ffn', [1, 1024])
    peer_w_q = din('peer_w_q', [1024, 2048])
    peer_keys1 = din('peer_keys1', [128, 128])
    peer_keys2 = din('peer_keys2', [128, 128])
    peer_u = din('peer_u', [16384, 1024])
    peer_v = din('peer_v', [16384, 1024])
    out_d = nc.dram_tensor('out', [TOK, 1024], F32, kind="ExternalOutput").ap()
    ysc = nc.dram_tensor('ysc', [TOK, 2048], F32, kind="Internal").ap()
    cb16 = nc.dram_tensor('cb16', [16384, 2048], BF16, kind="Internal").ap()
    b_ysc = [[Buf('ysc%d_%d' % (t, g)) for g in range(8)] for t in range(NT)]
    dbg_out = {}
    for name, shape in dbg:
        dbg_out[name] = nc.dram_tensor('dbg_' + name, shape, F32, kind="ExternalOutput").ap()

    top = ExitStack()

    def mk_sb(stack):
        def sb(name, shape, dt=F32):
            t = stack.enter_context(nc.sbuf_tensor('s_' + name, shape, dt))
            return t, Buf(name)
        return sb
    sb0 = mk_sb(top)

    psf = [(nc.alloc_psum_tensor('psf%d' % i, [128, 512], F32), Buf('psf%d' % i)) for i in range(6)]
    psb = [(nc.alloc_psum_tensor('psb%d' % i, [128, 1024], BF16), Buf('psb%d' % i)) for i in range(2)]
    rr = {'f': 0, 'b': 0, 'cast': 0, 'stg': 0}

    rr['n'] = 6

    def ps_next():
        rr['f'] = (rr['f'] + 1) % rr['n']
        return psf[rr['f']]

    def psb_next():
        rr['b'] = (rr['b'] + 1) % 2
        return psb[rr['b']]

    cst, b_cst = sb0('cst', [128, K_END])
    S.dma('sp', cst[:], cst_d, [], [b_cst], b_cst)
    cstb, b_cstb = sb0('cstb', [128, K_INVF], BF16)
    op('dve', lambda e: e.tensor_copy(out=cstb[:], in_=cst[:, 0:K_INVF]), [b_cst], [b_cstb])
    identf = cst[:, K_ID:K_ID + 128]
    identb = cstb[:, K_ID:K_ID + 128]
    flag, b_flag = sb0('flag', [128, 1])
    S.dma('sp', flag[:], flag_d, [], [b_flag], b_flag)
    epst, b_eps = sb0('epst', [128, 2])
    op('pool', lambda e: e.memset(epst[:, 0:1], EPS), [], [b_eps])
    op('pool', lambda e: e.memset(epst[:, 1:2], 1.0), [], [b_eps])
    stg = [sb0('stg%d' % i, [128, 2048]) for i in range(2)]

    wbufs = {}
    wq_ = {'q': 'sp'}

    def load_w(dst, src, nk, ncols, P=128, name='w'):
        bufs = []
        if ncols <= 2048:
            kstep = max(1, 2048 // ncols)
            cstep = ncols
        else:
            kstep = 1
            cstep = 2048
        for k0 in range(0, nk, kstep):
            k1 = min(nk, k0 + kstep)
            for c0 in range(0, ncols, cstep):
                c1 = min(ncols, c0 + cstep)
                n = (k1 - k0) * (c1 - c0)
                rr['stg'] ^= 1
                st, b_st = stg[rr['stg']]
                sv = st[0:P, 0:n].rearrange("p (k c) -> p k c", k=k1 - k0)
                S.dma(wq_['q'], sv, src[k0 * P:k1 * P, c0:c1].rearrange("(k p) c -> p k c", p=P), [], [b_st], b_st)
                bkey = (name, k0, c0)
                if bkey not in wbufs:
                    wbufs[bkey] = Buf('%s_%d_%d' % bkey)
                b = wbufs[bkey]
                rr['cast'] = (rr['cast'] + 1) % 3
                eng = ('pool', 'dve', 'act')[rr['cast']]
                dv = dst[0:P, k0:k1, c0:c1]
                if eng == 'act':
                    op(eng, lambda e, dv=dv, sv=sv: e.copy(out=dv, in_=sv), [b_st], [b])
                else:
                    op(eng, lambda e, dv=dv, sv=sv: e.tensor_copy(out=dv, in_=sv), [b_st], [b])
                bufs.append(b)
        return bufs

    def bcast_load(sbf, name, src, n):
        t, b = sbf(name, [128, n])
        S.dma('sp', t[:], src.partition_broadcast(128), [], [b], b)
        return t, b

    def rstd_from_ssq(ssq_ap, out_ap, n, rbuf):
        op('act', lambda e: e.activation(out=out_ap, in_=ssq_ap, func=AF.Sqrt, scale=1.0 / n, bias=epst[:, 0:1]), [rbuf, b_eps], [rbuf])
        op('dve', lambda e: e.reciprocal(out=out_ap, in_=out_ap), [rbuf], [rbuf])

    def dump(name, src_ap, rbuf):
        if name in dbg_out:
            S.dma('sp', dbg_out[name], src_ap, [rbuf], [], rbuf)

    msc = nc.dram_tensor('msc', [TOK, 1024], F32, kind="Internal").ap()
    b_msc = [Buf('msc%d' % t) for t in range(NT)]
    b_x1d = [Buf('x1d%d' % t) for t in range(NT)]

    mid = ExitStack()
    sb1 = mk_sb(mid)
    hTo, _ = sb1('hTo', [128, 8, TOK], BF16)
    ctxs = ExitStack()
    sbc = mk_sb(ctxs)
    hTc, _ = sbc('hTc', [128, 8, TOK], BF16)
    b_hT = [Buf('hT%d' % t) for t in range(32)]
    own = lambda t: slice(TOK + t * 128, TOK + (t + 1) * 128)

    def hsl(kc, sl):
        step = sl.step or 1
        if sl.start >= TOK:
            return hTo[:, kc, sl.start - TOK:sl.stop - TOK:step]
        assert sl.stop <= TOK
        return hTc[:, kc, sl.start:sl.stop:step]

    with ExitStack() as ph:
        sb = mk_sb(ph)
        gmix, b_gmix = bcast_load(sb, 'gmix', norm_mix, 1024)
        xts = [sb('xt%d' % i, [128, 4, 1024]) for i in range(2)]
        sq, b_sq = sb('sq', [128, 1024])
        hbr = [sb('hb%d' % i, [128, 1024], BF16) for i in range(2)]
        str_ = [sb('st%d' % i, [128, 2]) for i in range(2)]
        xa4 = xa.rearrange("(n j p) d -> n p j d", j=4, p=128)
        pend = []

        def stage1(tt):
            xt4, b_xt = xts[(tt // 4) % 2]
            if tt % 4 == 0:
                S.dma('sp', xt4[:], xa4[tt // 4], [], [b_xt], b_xt)
            xt = xt4[:, tt % 4, :]
            hb, b_hb = hbr[tt % 2]
            st, b_st = str_[tt % 2]
            op('act', lambda e: e.activation(out=sq[:], in_=xt, func=AF.Square, accum_out=st[:, 0:1]), [b_xt], [b_sq, b_st])
            rstd_from_ssq(st[:, 0:1], st[:, 1:2], 1024, b_st)
            op('dve', lambda e: e.scalar_tensor_tensor(out=hb[:], in0=xt, scalar=st[:, 1:2], in1=gmix[:], op0=ALU.mult, op1=ALU.mult),
               [b_xt, b_st, b_gmix], [b_hb])
            pb, b_pb = psb_next()
            for kc in range(8):
                op('pe', lambda e, kc=kc: e.transpose(out=pb[:, kc * 128:(kc + 1) * 128], in_=hb[:, kc * 128:(kc + 1) * 128], identity=identb),
                   [b_hb, b_cstb], [b_pb])
            pend.append((tt, pb, b_pb))

        def stage2():
            tt, pb, b_pb = pend.pop(0)
            hdst = hTc[:, :, tt * 128:(tt + 1) * 128] if tt < 16 else hTo[:, :, (tt - 16) * 128:(tt - 15) * 128]
            op('act', lambda e: e.copy(out=hdst, in_=pb[:].rearrange("p (k t) -> p k t", k=8)), [b_pb], [b_hT[tt]])
        stage1(0)
        for tt in range(1, 32):
            stage1(tt)
            stage2()
        stage2()
        S.barrier()
    if 'hT' in dbg_out:
        with ExitStack() as ph:
            sb = mk_sb(ph)
            tmp, b_tmp = sb('dbgt', [128, 4096])
            op('dve', lambda e: e.tensor_copy(out=tmp[:, 0:TOK], in_=hTc[:, 3, :]), b_hT, [b_tmp])
            op('dve', lambda e: e.tensor_copy(out=tmp[:, TOK:], in_=hTo[:, 3, :]), b_hT, [b_tmp])
            dump('hT', tmp[:], b_tmp)
            S.barrier()

    if upto >= 1:
        dts = ExitStack()
        sbd = mk_sb(dts)
        dtt, b_dtt = sbd('dtt', [128, 32, 32])
        at, b_at = sbd('at', [128, 32, 32])
        ecs, b_ecs = sbd('ecs', [128, 32, 32])
        etot, b_etot = sbd('etot', [128, 32, 32])
        wst, b_wst = sbd('wst', [128, 32, 32])
        dskb, b_dskb = bcast_load(sbd, 'dskb', d_skip, 32)
        with ExitStack() as ph:
            sb = mk_sb(ph)
            wdt, _ = sb('wdt', [128, 8, 32], BF16)
            bw = load_w(wdt, w_in[:, C_DT:C_DT + 32], 8, 32, name='wdt')
            dtbb, b_dtbb = bcast_load(sb, 'dtbb', dt_bias, 32)
            alb, b_alb = bcast_load(sb, 'alb', a_log, 32)
            t0, b_t0 = sb('dt_t0', [128, 32, 32])
            t1, b_t1 = sb('dt_t1', [128, 32, 32])
            op('act', lambda e: e.activation(out=alb[:], in_=alb[:], func=AF.Exp), [b_alb], [b_alb])
            for half in range(2):
                p, b_p = ps_next()
                for j in range(16):
                    tt = half * 16 + j
                    for kc in range(8):
                        op('pe', lambda e, p=p, j=j, tt=tt, kc=kc: e.matmul(p[:, j * 32:(j + 1) * 32], lhsT=hsl(kc, slice(tt * 128, (tt + 1) * 128)),
                                                                           rhs=wdt[:, kc, :], start=(kc == 0), stop=(kc == 7)),
                           [b_hT[tt]] + bw, [b_p])
                op('dve', lambda e, p=p, half=half: e.tensor_tensor(out=t0[:, half * 16:(half + 1) * 16, :], in0=p[:].rearrange("p (j h) -> p j h", j=16),
                                                                    in1=dtbb[:].unsqueeze(1).to_broadcast([128, 16, 32]), op=ALU.add),
                   [b_p, b_dtbb], [b_t0])
            op('act', lambda e: e.activation(out=t1[:], in_=t0[:], func=AF.Abs), [b_t0], [b_t1])
            op('act', lambda e: e.activation(out=t1[:], in_=t1[:], func=AF.Exp, scale=-1.0), [b_t1], [b_t1])
            op('act', lambda e: e.activation(out=t1[:], in_=t1[:], func=AF.Ln, bias=epst[:, 1:2]), [b_t1, b_eps], [b_t1])
            op('dve', lambda e: e.scalar_tensor_tensor(out=dtt[:], in0=t0[:], scalar=0.0, in1=t1[:], op0=ALU.max, op1=ALU.add), [b_t0, b_t1], [b_dtt])
            op('dve', lambda e: e.scalar_tensor_tensor(out=at[:], in0=dtt[:], scalar=-1.0, in1=alb[:].unsqueeze(1).to_broadcast([128, 32, 32]),
                                                       op0=ALU.mult, op1=ALU.mult), [b_dtt, b_alb], [b_at])
            atf = at[:].rearrange("p c h -> p (c h)")
            for half in range(2):
                sl = slice(half * 512, (half + 1) * 512)
                pc, b_pc = ps_next()
                op('pe', lambda e, pc=pc, sl=sl: e.matmul(pc[:], lhsT=cst[:, K_TRIU:K_TRIU + 128], rhs=atf[:, sl], start=True, stop=True), [b_at, b_cst], [b_pc])
                pt, b_pt = ps_next()
                op('pe', lambda e, pt=pt, sl=sl: e.matmul(pt[:], lhsT=cst[:, K_ONES:K_ONES + 128], rhs=atf[:, sl], start=True, stop=True), [b_at, b_cst], [b_pt])
                t0f = t0[:].rearrange("p c h -> p (c h)")
                op('act', lambda e, pc=pc, sl=sl: e.activation(out=ecs[:].rearrange("p c h -> p (c h)")[:, sl], in_=pc[:], func=AF.Exp), [b_pc], [b_ecs])
                op('act', lambda e, pt=pt, sl=sl: e.activation(out=etot[:].rearrange("p c h -> p (c h)")[:, sl], in_=pt[:], func=AF.Exp), [b_pt], [b_etot])
                op('act', lambda e, pc=pc, sl=sl, t0f=t0f: e.copy(out=t0f[:, sl], in_=pc[:]), [b_pc, b_t0], [b_t0])
                op('dve', lambda e, pt=pt, sl=sl, t0f=t0f: e.tensor_tensor(out=t0f[:, sl], in0=pt[:], in1=t0f[:, sl], op=ALU.subtract), [b_pt, b_t0], [b_t0])
            op('act', lambda e: e.activation(out=t0[:], in_=t0[:], func=AF.Exp), [b_t0], [b_t0])
            op('dve', lambda e: e.tensor_tensor(out=wst[:], in0=t0[:], in1=dtt[:], op=ALU.mult), [b_t0, b_dtt], [b_wst])
            dump('dtt', dtt[:].rearrange("p c h -> p (c h)"), b_dtt)
            dump('ecs', ecs[:].rearrange("p c h -> p (c h)"), b_ecs)
            dump('wst', wst[:].rearrange("p c h -> p (c h)"), b_wst)
            S.barrier()

    if upto >= 2:
        with ExitStack() as ph:
            sb = mk_sb(ph)
            cwr, b_cwr = sb('cwr', [128, 128])
            S.dma('sp', cwr[:], conv_w.rearrange("k (n p) -> (k n) p", p=128), [], [b_cwr], b_cwr)
            cbr, b_cbr = sb('cbr', [32, 128])
            S.dma('sp', cbr[:], conv_b.rearrange("o (n p) -> (o n) p", p=128), [], [b_cbr], b_cbr)
            cwT, b_cwT = sb('cwT', [128, 4, 32])
            cbT, b_cbT = sb('cbT', [128, 32])
            p, b_p = ps_next()
            op('pe', lambda e, p=p: e.transpose(out=p[:, 0:128], in_=cwr[:], identity=identf), [b_cwr, b_cst], [b_p])
            op('pe', lambda e, p=p: e.transpose(out=p[:, 128:160], in_=cbr[:], identity=cst[0:32, K_ID:K_ID + 32]), [b_cbr, b_cst], [b_p])
            op('act', lambda e, p=p: e.copy(out=cwT[:].rearrange("p k n -> p (k n)"), in_=p[:, 0:128]), [b_p], [b_cwT])
            op('act', lambda e, p=p: e.copy(out=cbT[:], in_=p[:, 128:160]), [b_p], [b_cbT])
            wg, _ = sb('wg', [128, 8, 512], BF16)
            raws = [sb('raw%d' % j, [128, 515]) for j in range(4)]
            cacc, b_cacc = sb('cacc', [128, 512])
            xbcT, _ = sb('xbcT', [128, 4, 4096], BF16)
            b_xbc = [[Buf('xbc%d_%d' % (j, s)) for s in range(8)] for j in range(4)]
            xtokr = [sb('xtok%d' % i, [128, 384], BF16) for i in range(3)]
            xsr = [sb('xs%d' % i, [128, 256], BF16) for i in range(3)]
            xdr = [sb('xd%d' % i, [128, 256], BF16) for i in range(3)]
            cbmr = [sb('cbm%d' % i, [128, 128]) for i in range(3)]
            arhsr = [sb('arhs%d' % i, [128, 4, 128]) for i in range(2)]
            decr = [sb('dec%d' % i, [128, 4, 128]) for i in range(2)]
            MTr = [sb('MT%d' % i, [128, 4, 128], BF16) for i in range(3)]
            t2sr = [sb('t2s%d' % i, [128, 256]) for i in range(3)]
            t1sr = [sb('t1s%d' % i, [128, 256]) for i in range(2)]
            ybufs = [sb('ybuf%d' % i, [128, 256]) for i in range(2)]
            Sp, b_Sp = sb('Sprev', [128, 256])
            Stmp, b_Stmp = sb('Stmp', [128, 256])
            Sbfr = [sb('Sbf%d' % i, [128, 256], BF16) for i in range(2)]
            triu_f = cst[:, K_TRIU:K_TRIU + 128]
            NCV = 3
            cvi = [sb('cvi%d' % i, [128, 1024]) for i in range(NCV)]
            cvo = [sb('cvo%d' % i, [128, 1024], BF16) for i in range(NCV)]
            b_cb16 = Buf('cb16')
            tabs = (peer_u.rearrange("(p r) d -> p r d", r=128), peer_v.rearrange("(p r) d -> p r d", r=128))
            c_v = cb16.rearrange("(p r) d -> p r d", r=128)

            def convert_step(q):
                if q < 256:
                    ci_, b_ci = cvi[q % NCV]
                    S.dma('sp', ci_[:], tabs[q % 2][:, q // 2, :], [], [b_ci], b_ci)
                if 0 <= q - 1 < 256:
                    ci_, b_ci = cvi[(q - 1) % NCV]
                    co_, b_co = cvo[(q - 1) % NCV]
                    op('act', lambda e: e.copy(out=co_[:], in_=ci_[:]), [b_ci], [b_co])
                if 0 <= q - 2 < 256:
                    co_, b_co = cvo[(q - 2) % NCV]
                    w_ = (q - 2) % 2
                    S.dma('sp', c_v[:, (q - 2) // 2, w_ * 1024:(w_ + 1) * 1024], co_[:], [b_co], [b_cb16], b_co)
            conv_r = [0]
            for g in range(8):
                bw = load_w(wg[:, :, 0:256], w_in[:, C_X + g * 256:C_X + (g + 1) * 256], 8, 256, name='wgx')
                bw += load_w(wg[:, :, 256:384], w_in[:, C_B + g * 128:C_B + (g + 1) * 128], 8, 128, name='wgb')
                bw += load_w(wg[:, :, 384:512], w_in[:, C_C + g * 128:C_C + (g + 1) * 128], 8, 128, name='wgc')
                nidx = [2 * g, 2 * g + 1, 16 + g, 24 + g]
                for j in range(4):
                    raw, b_raw = raws[j]
                    op('pool', lambda e, raw=raw: e.memset(raw[:, 0:3], 0.0), [], [b_raw])
                op('pool', lambda e: e.memset(Sp[:], 0.0), [], [b_Sp])
                op('pool', lambda e: e.memset(Sbfr[0][0][:], 0.0), [], [Sbfr[0][1]])
                for s in range(8):
                    for j in range(4):
                        if j == 3 and s < 3:
                            continue
                        raw, b_raw = raws[j]
                        n = nidx[j]
                        p, b_p = ps_next()
                        for kc in range(8):
                            op('pe', lambda e, p=p, kc=kc, j=j, s=s: e.matmul(p[:], lhsT=wg[:, kc, j * 128:(j + 1) * 128], rhs=hsl(kc, slice(s * 512, (s + 1) * 512)),
                                                                           start=(kc == 0), stop=(kc == 7)),
                               b_hT[4 * s:4 * s + 4] + bw, [b_p])
                        op('act', lambda e, p=p, raw=raw: e.copy(out=raw[:, 3:515], in_=p[:]), [b_p], [b_raw])
                        op('dve', lambda e, raw=raw, n=n: e.tensor_scalar(out=cacc[:], in0=raw[:, 3:515], scalar1=cwT[:, 3, n:n + 1], scalar2=cbT[:, n:n + 1],
                                                                         op0=ALU.mult, op1=ALU.add), [b_raw, b_cwT, b_cbT], [b_cacc])
                        for k in range(3):
                            op('dve', lambda e, raw=raw, n=n, k=k: e.scalar_tensor_tensor(out=cacc[:], in0=raw[:, k:k + 512], scalar=cwT[:, k, n:n + 1], in1=cacc[:],
                                                                                          op0=ALU.mult, op1=ALU.add), [b_raw, b_cwT, b_cacc], [b_cacc])
                        op('act', lambda e, j=j, s=s: e.activation(out=xbcT[:, j, s * 512:(s + 1) * 512], in_=cacc[:], func=AF.Silu), [b_cacc], [b_xbc[j][s]])
                        op('pool', lambda e, raw=raw: e.tensor_copy(out=raw[:, 0:3], in_=raw[:, 512:515]), [b_raw], [b_raw])
                if g == 0:
                    for j in range(4):
                        if ('xbc%d' % j) in dbg_out:
                            tmpd, b_tmpd = sb('dbgx%d' % j, [128, 4096])
                            op('dve', lambda e, j=j, tmpd=tmpd: e.tensor_copy(out=tmpd[:], in_=xbcT[:, j, :]), b_xbc[j], [b_tmpd])
                            dump('xbc%d' % j, tmpd[:], b_tmpd)
                rr['n'] = 4
                hs = slice(4 * g, 4 * g + 4)
                pSr = {}

                def frontA(c):
                    s_ = c // 4
                    tk = slice(c * 128, (c + 1) * 128)
                    xtok, b_xtok = xtokr[c % 3]
                    pb, b_pb = psb_next()
                    if c >= 16:
                        arhs, b_arhs = arhsr[c % 2]
                        op('pool', lambda e: e.tensor_tensor(out=arhs[:], in0=triu_f.unsqueeze(1).to_broadcast([128, 4, 128]),
                                                             in1=at[:, c, hs].unsqueeze(2).to_broadcast([128, 4, 128]), op=ALU.mult),
                           [b_cst, b_at], [b_arhs])
                    for j in range(3):
                        op('pe', lambda e, j=j: e.transpose(out=pb[:, j * 128:(j + 1) * 128], in_=xbcT[:, j, tk], identity=identb),
                           [b_xbc[j][s_], b_cstb], [b_pb])
                    op('act', lambda e: e.copy(out=xtok[:], in_=pb[:, 0:384]), [b_pb], [b_xtok])
                    if c >= 16:
                        dec, b_dec = decr[c % 2]
                        pG, b_pG = ps_next()
                        op('pe', lambda e: e.matmul(pG[:], lhsT=cst[:, K_TRILS:K_TRILS + 128], rhs=arhs[:].rearrange("p h l -> p (h l)"), start=True, stop=True),
                           [b_arhs, b_cst], [b_pG])
                        op('act', lambda e: e.activation(out=dec[:].rearrange("p h l -> p (h l)"), in_=pG[:], func=AF.Exp), [b_pG], [b_dec])
                    if conv_r[0] < 258:
                        convert_step(conv_r[0])
                        conv_r[0] += 1

                def frontB(c):
                    s_ = c // 4
                    tk = slice(c * 128, (c + 1) * 128)
                    xtok, b_xtok = xtokr[c % 3]
                    xs, b_xs = xsr[c % 3]
                    xv = xtok[:, 0:256].rearrange("p (h d) -> p h d", h=4)
                    op('dve', lambda e: e.tensor_tensor(out=xs[:].rearrange("p (h d) -> p h d", h=4), in0=xv,
                                                         in1=wst[:, c, hs].unsqueeze(2).to_broadcast([128, 4, 64]), op=ALU.mult),
                       [b_xtok, b_wst], [b_xs])
                    pS, b_pS = psf[4 + c % 2]
                    pSr[c] = (pS, b_pS)
                    op('pe', lambda e: e.matmul(pS[:, 0:256], lhsT=xtok[:, 256:384], rhs=xs[:], start=True, stop=True), [b_xtok, b_xs], [b_pS])
                    if c >= 16:
                        xd, b_xd = xdr[c % 3]
                        cbm, b_cbm = cbmr[c % 3]
                        dec, b_dec = decr[c % 2]
                        MT, b_MT = MTr[c % 3]
                        t2s, b_t2s = t2sr[c % 3]
                        op('dve', lambda e: e.tensor_tensor(out=xd[:].rearrange("p (h d) -> p h d", h=4), in0=xv,
                                                             in1=dtt[:, c, hs].unsqueeze(2).to_broadcast([128, 4, 64]), op=ALU.mult),
                           [b_xtok, b_dtt], [b_xd])
                        pC, b_pC = ps_next()
                        op('pe', lambda e: e.matmul(pC[:, 0:128], lhsT=xbcT[:, 2, tk], rhs=xbcT[:, 3, tk], start=True, stop=True),
                           [b_xbc[2][s_], b_xbc[3][s_]], [b_pC])
                        op('dve', lambda e: e.tensor_tensor(out=cbm[:], in0=pC[:, 0:128], in1=triu_f, op=ALU.mult), [b_pC, b_cst], [b_cbm])
                        op('dve', lambda e: e.tensor_tensor(out=MT[:], in0=dec[:], in1=cbm[:].unsqueeze(1).to_broadcast([128, 4, 128]), op=ALU.mult),
                           [b_dec, b_cbm], [b_MT])
                        op('pool', lambda e: e.tensor_tensor(out=t2s[:].rearrange("p (h d) -> p h d", h=4), in0=xv,
                                                             in1=dskb[:, hs].unsqueeze(2).to_broadcast([128, 4, 64]), op=ALU.mult),
                           [b_xtok, b_dskb], [b_t2s])

                def tail(c):
                    s_ = c // 4
                    tk = slice(c * 128, (c + 1) * 128)
                    pS, b_pS = pSr.pop(c)
                    Sbf, b_Sbf = Sbfr[c % 2]
                    Sbn, b_Sbn = Sbfr[(c + 1) % 2]
                    op('dve', lambda e: e.tensor_tensor(out=Stmp[:].rearrange("p (h d) -> p h d", h=4), in0=Sp[:].rearrange("p (h d) -> p h d", h=4),
                                                        in1=etot[:, c, hs].unsqueeze(2).to_broadcast([128, 4, 64]), op=ALU.mult),
                       [b_Sp, b_etot], [b_Stmp])
                    if c == 15:
                        op('dve', lambda e: e.tensor_tensor(out=Stmp[:], in0=pS[:, 0:256], in1=Stmp[:], op=ALU.add), [b_pS, b_Stmp], [b_Stmp])
                        op('dve', lambda e: e.tensor_scalar(out=Sp[:], in0=Stmp[:], scalar1=flag[:, 0:1], scalar2=None, op0=ALU.mult), [b_Stmp, b_flag], [b_Sp])
                    else:
                        op('dve', lambda e: e.tensor_tensor(out=Sp[:], in0=pS[:, 0:256], in1=Stmp[:], op=ALU.add), [b_pS, b_Stmp], [b_Sp])
                    op('act', lambda e: e.copy(out=Sbn[:], in_=Sp[:]), [b_Sp], [b_Sbn])
                    if c >= 16:
                        t = c - 16
                        xd, b_xd = xdr[c % 3]
                        MT, b_MT = MTr[c % 3]
                        t2s, b_t2s = t2sr[c % 3]
                        t1s, b_t1s = t1sr[c % 2]
                        pY, b_pY = ps_next()
                        for h in range(4):
                            op('pe', lambda e, h=h: e.matmul(pY[:, h * 64:(h + 1) * 64], lhsT=MT[:, h, :], rhs=xd[:, h * 64:(h + 1) * 64], start=True, stop=True),
                               [b_MT, b_xd], [b_pY])
                        pO, b_pO = ps_next()
                        op('pe', lambda e: e.matmul(pO[:, 0:256], lhsT=xbcT[:, 3, tk], rhs=Sbf[:], start=True, stop=True), [b_xbc[3][s_], b_Sbf], [b_pO])
                        op('dve', lambda e: e.tensor_tensor(out=t1s[:].rearrange("p (h d) -> p h d", h=4), in0=pO[:, 0:256].rearrange("p (h d) -> p h d", h=4),
                                                            in1=ecs[:, c, hs].unsqueeze(2).to_broadcast([128, 4, 64]), op=ALU.mult),
                           [b_pO, b_ecs], [b_t1s])
                        yb, b_yb = ybufs[t % 2]
                        op('dve', lambda e: e.tensor_tensor(out=t1s[:], in0=pY[:, 0:256], in1=t1s[:], op=ALU.add), [b_pY, b_t1s], [b_t1s])
                        op('pool', lambda e: e.tensor_tensor(out=yb[:], in0=t1s[:], in1=t2s[:], op=ALU.add), [b_t1s, b_t2s], [b_yb])
                        S.dma('sp', ysc[t * 128:(t + 1) * 128, g * 256:(g + 1) * 256], yb[:], [b_yb], [b_ysc[t][g]], b_yb)

                frontA(0)
                frontA(1)
                frontB(0)
                for c in range(32):
                    tail(c)
                    if c + 1 < 32:
                        frontB(c + 1)
                    if c + 2 < 32:
                        frontA(c + 2)
                rr['n'] = 6
            while conv_r[0] < 258:
                convert_step(conv_r[0])
                conv_r[0] += 1
            S.barrier()

    def gate_and_merge(t, pU, b_pU, wg_t, bwg, first, sg, b_sg, tmpm, b_tmpm, mtile, b_mtile):
        if not first:
            S.dma('sp', mtile[:], msc[t * 128:(t + 1) * 128, :], [b_msc[t]], [b_mtile], b_mtile)
        for nb in range(2):
            pg, b_pg = ps_next()
            for kc in range(8):
                op('pe', lambda e, pg=pg, kc=kc, nb=nb: e.matmul(pg[:], lhsT=hsl(kc, own(t)), rhs=wg_t[:, kc, nb * 512:(nb + 1) * 512], start=(kc == 0), stop=(kc == 7)),
                   [b_hT[16 + t]] + bwg, [b_pg])
            op('act', lambda e, pg=pg, nb=nb: e.activation(out=sg[:, nb * 512:(nb + 1) * 512], in_=pg[:], func=AF.Sigmoid), [b_pg], [b_sg])
            op('dve', lambda e, nb=nb: e.tensor_tensor(out=tmpm[:, nb * 512:(nb + 1) * 512], in0=pU[nb][:], in1=sg[:, nb * 512:(nb + 1) * 512], op=ALU.mult),
               [b_pU[nb], b_sg], [b_tmpm])
        if not first:
            op('pool', lambda e: e.tensor_tensor(out=tmpm[:], in0=tmpm[:], in1=mtile[:], op=ALU.add), [b_tmpm, b_mtile], [b_tmpm])
        S.dma('sp', msc[t * 128:(t + 1) * 128, :], tmpm[:], [b_tmpm], [b_msc[t]], b_tmpm)

    def phase_ssd_final():
        with ExitStack() as ph:
            sb = mk_sb(ph)
            wz, _ = sb('wz', [128, 8, 2048], BF16)
            bwz = load_w(wz, w_in[:, C_Z:C_Z + 2048], 8, 2048, name='wz')
            wg0, _ = sb('wg0', [128, 8, 1024], BF16)
            bwg0 = load_w(wg0, w_in[:, C_G:C_G + 1024], 8, 1024, name='wg0')
            wus, _ = sb('wus', [128, 16, 1024], BF16)
            bwus = load_w(wus, w_up_ssd, 16, 1024, name='wus')
            gssd, b_gssd = bcast_load(sb, 'gssd', ssd_norm, 2048)
            ytr = [sb('yt%d' % i, [128, 2048]) for i in range(2)]
            szr = [sb('sz%d' % i, [128, 2048]) for i in range(2)]
            ynr = [sb('yn%d' % i, [128, 2048], BF16) for i in range(2)]
            yTr = [sb('yT%d' % i, [128, 16, 128], BF16) for i in range(2)]
            str3 = [sb('st3_%d' % i, [128, 2]) for i in range(2)]
            sg, b_sg = sb('sg', [128, 1024])
            tmpm, b_tmpm = sb('tmpm3', [128, 1024])
            mtile, b_mtile = sb('mtile3', [128, 1024])
            live = {}

            def stA(t):
                yt, b_yt = ytr[t % 2]
                sz, b_sz = szr[t % 2]
                S.dma('sp', yt[:], ysc[t * 128:(t + 1) * 128, :], b_ysc[t], [b_yt], b_yt)
                for nb in range(4):
                    p, b_p = ps_next()
                    for kc in range(8):
                        op('pe', lambda e, kc=kc, nb=nb: e.matmul(p[:], lhsT=hsl(kc, own(t)), rhs=wz[:, kc, nb * 512:(nb + 1) * 512], start=(kc == 0), stop=(kc == 7)),
                           [b_hT[16 + t]] + bwz, [b_p])
                    op('act', lambda e, nb=nb: e.activation(out=sz[:, nb * 512:(nb + 1) * 512], in_=p[:], func=AF.Silu), [b_p], [b_sz])

            def stB(t):
                yt, b_yt = ytr[t % 2]
                sz, b_sz = szr[t % 2]
                yn, b_yn = ynr[t % 2]
                st, b_st = str3[t % 2]
                if t == 0:
                    dump('yraw', yt[:], b_yt)
                op('dve', lambda e: e.tensor_tensor(out=yt[:], in0=yt[:], in1=sz[:], op=ALU.mult), [b_yt, b_sz], [b_yt])
                op('act', lambda e: e.activation(out=sz[:], in_=yt[:], func=AF.Square, accum_out=st[:, 0:1]), [b_yt], [b_sz, b_st])
                rstd_from_ssq(st[:, 0:1], st[:, 1:2], 2048, b_st)
                op('dve', lambda e: e.scalar_tensor_tensor(out=yn[:], in0=yt[:], scalar=st[:, 1:2], in1=gssd[:], op0=ALU.mult, op1=ALU.mult),
                   [b_yt, b_st, b_gssd], [b_yn])
                pbs = []
                for half in range(2):
                    pb, b_pb = psb_next()
                    for k in range(8):
                        kc = half * 8 + k
                        op('pe', lambda e, k=k, kc=kc: e.transpose(out=pb[:, k * 128:(k + 1) * 128], in_=yn[:, kc * 128:(kc + 1) * 128], identity=identb),
                           [b_yn, b_cstb], [b_pb])
                    pbs.append((pb, b_pb))
                live[t] = pbs

            def stC(t):
                yT, b_yT = yTr[t % 2]
                for half, (pb, b_pb) in enumerate(live.pop(t)):
                    op('act', lambda e, half=half: e.copy(out=yT[:, half * 8:(half + 1) * 8, :], in_=pb[:].rearrange("p (k t) -> p k t", k=8)), [b_pb], [b_yT])
                pU = []
                b_pU = []
                for nb in range(2):
                    p, b_p = ps_next()
                    for kc in range(16):
                        op('pe', lambda e, kc=kc, nb=nb: e.matmul(p[:], lhsT=yT[:, kc, :], rhs=wus[:, kc, nb * 512:(nb + 1) * 512], start=(kc == 0), stop=(kc == 15)),
                           [b_yT] + bwus, [b_p])
                    pU.append(p)
                    b_pU.append(b_p)
                gate_and_merge(t, pU, b_pU, wg0, bwg0, False, sg, b_sg, tmpm, b_tmpm, mtile, b_mtile)
            stages = (stA, stB, stC)
            for step in range(NT + 2):
                for j in (2, 1, 0):
                    t = step - j
                    if 0 <= t < NT:
                        stages[j](t)
            S.barrier()

    def phase_dil():
        with ExitStack() as ph:
            sb = mk_sb(ph)
            ydT, b_ydT = sb('ydT', [128, 4, TOK], BF16)
            gqc, b_gqc = sb('gqc', [128, 1])
            gkc, b_gkc = sb('gkc', [128, 1])
            S.dma('sp', gqc[:], gq_col, [], [b_gqc], b_gqc)
            S.dma('sp', gkc[:], gk_col, [], [b_gkc], b_gkc)
            NH = 2
            with ExitStack() as ph2:
                sb2 = mk_sb(ph2)
                cosF, b_cosF = sb2('cosF', [128, 4096], BF16)
                sinF, b_sinF = sb2('sinF', [128, 4096], BF16)
                with ExitStack() as ph3:
                    sb3 = mk_sb(ph3)
                    posb, b_posb = sb3('posb', [128, 2048], I32)
                    angF, b_angF = sb3('angF', [128, 2048])
                    tmpF, b_tmpF = sb3('tmpF', [128, 2048])
                    kiF, b_kiF = sb3('kiF', [128, 2048], I32)
                    kfF, b_kfF = sb3('kfF', [128, 2048])
                    TWO_PI = 2 * math.pi
                    PCL = 3.141592
                    posa2 = posa.rearrange("(o n) -> o n", o=1)
                    for hf_ in range(2):
                        tsl_ = slice(hf_ * 2048, (hf_ + 1) * 2048)
                        S.dma('sp', posb[:], posa2[:, tsl_].partition_broadcast(128), [], [b_posb], b_posb)
                        op('dve', lambda e: e.tensor_copy(out=angF[:], in_=posb[:]), [b_posb], [b_angF])
                        op('dve', lambda e: e.tensor_scalar(out=angF[:], in0=angF[:], scalar1=cst[:, K_INVFC:K_INVFC + 1], scalar2=None, op0=ALU.mult), [b_angF, b_cst], [b_angF])
                        for dst, b_dst, shift in ((sinF, b_sinF, 0.0), (cosF, b_cosF, 0.5 * math.pi)):
                            op('dve', lambda e: e.tensor_scalar(out=tmpF[:], in0=angF[:], scalar1=shift, scalar2=None, op0=ALU.add), [b_angF], [b_tmpF])
                            op('dve', lambda e: e.tensor_scalar(out=kiF[:], in0=tmpF[:], scalar1=1.0 / TWO_PI, scalar2=None, op0=ALU.mult), [b_tmpF], [b_kiF])
                            op('dve', lambda e: e.tensor_copy(out=kfF[:], in_=kiF[:]), [b_kiF], [b_kfF])
                            op('dve', lambda e: e.scalar_tensor_tensor(out=tmpF[:], in0=kfF[:], scalar=-TWO_PI, in1=tmpF[:], op0=ALU.mult, op1=ALU.add), [b_kfF, b_tmpF], [b_tmpF])
                            op('dve', lambda e: e.tensor_scalar(out=tmpF[:], in0=tmpF[:], scalar1=-PCL, scalar2=PCL, op0=ALU.max, op1=ALU.min), [b_tmpF], [b_tmpF])
                            op('act', lambda e: e.activation(out=dst[:, tsl_], in_=tmpF[:], func=AF.Sin), [b_tmpF], [b_dst])
                    S.barrier()
                if 'cosF' in dbg_out:
                    tmpd, b_tmpd = sb2('dbgc', [128, 4096])
                    op('dve', lambda e: e.tensor_copy(out=tmpd[:], in_=cosF[:]), [b_cosF], [b_tmpd])
                    dump('cosF', tmpd[:], b_tmpd)
                if DIL_STOP <= 1:
                    S.barrier()
                    return
                acc, b_acc = sb2('acc', [65, NH, TOK])
                wd, _ = sb2('wd', [128, 8, 384], BF16)
                kTf, _ = sb2('kTf', [128, 4096], BF16)
                qTf, _ = sb2('qTf', [128, TOK], BF16)
                b_kTf = [Buf('kTf%d' % i) for i in range(8)]
                b_qTf = [Buf('qTf%d' % i) for i in range(4)]
                Vt, _ = sb2('Vt', [128, 32, NH, 65], BF16)
                vTf, _ = sb2('vTf', [128, 4096], BF16)
                b_vTf = [Buf('vTf%d' % i) for i in range(8)]
                b_Vt = [Buf('Vt%d' % i) for i in range(32)]
                b_Vones = Buf('Vones')
                sqbr = [sb2('sqb%d' % i, [128, 512], BF16) for i in range(2)]
                rsr = [sb2('rs%d' % i, [128, 512]) for i in range(2)]
                qnr = [sb2('qn%d' % i, [128, 512], BF16) for i in range(2)]
                rar = [sb2('ra%d' % i, [128, 512]) for i in range(2)]
                rbr = [sb2('rb%d' % i, [128, 512]) for i in range(2)]
                pTr = [sb2('pT%d' % i, [128, 4, 128], BF16) for i in range(3)]
                maskc, b_maskc = sb2('maskc', [128, 4, 128], BF16)
                rec, b_rec = sb2('rec', [128, 512])
                op('pool', lambda e: e.memset(Vt[:, :, :, 64:65], 1.0), [], [b_Vones])
                for i_ in range(4):
                    src_ = cstb[:, K_TRIL:K_TRIL + 128] if i_ % 2 == 0 else cstb[:, K_TRIU:K_TRIU + 128]
                    op('pool', lambda e: e.tensor_copy(out=maskc[:, i_, :], in_=src_), [b_cstb, b_maskc], [b_maskc])
                Bm = cstb[:, K_BM:K_BM + 128]
                Rm = cstb[:, K_RM:K_RM + 128]
                uctr = [0]

                def qk_stages(wcol, gcol, b_gcol, start, n, dstT, dst_off, b_dst, bw):
                    u = uctr[0] % 2
                    uctr[0] += 1
                    sqb, b_sqb = sqbr[u]
                    rs, b_rs = rsr[u]
                    qn, b_qn = qnr[u]
                    ra, b_ra = rar[u]
                    rb, b_rb = rbr[u]
                    hb_ = [b_hT[x] for x in range(start // 128, (start + n - 1) // 128 + 1)]
                    st = {}

                    def stA():
                        p1, b_p1 = ps_next()
                        st['p1'] = (p1, b_p1)
                        for kc in range(8):
                            op('pe', lambda e, kc=kc: e.matmul(p1[:, 0:n], lhsT=wd[:, kc, wcol:wcol + 128], rhs=hsl(kc, slice(start, start + n)), start=(kc == 0), stop=(kc == 7)),
                               hb_ + bw, [b_p1])
                        op('act', lambda e: e.activation(out=sqb[:, 0:n], in_=p1[:, 0:n], func=AF.Square), [b_p1], [b_sqb])

                    def stB():
                        p1, b_p1 = st['p1']
                        p2, b_p2 = ps_next()
                        op('pe', lambda e: e.matmul(p2[:, 0:n], lhsT=Bm, rhs=sqb[:, 0:n], start=True, stop=True), [b_sqb, b_cstb], [b_p2])
                        op('act', lambda e: e.activation(out=rs[:, 0:n], in_=p2[:, 0:n], func=AF.Sqrt, scale=1.0 / 64, bias=epst[:, 0:1]), [b_p2, b_eps], [b_rs])
                        op('dve', lambda e: e.reciprocal(out=rs[:, 0:n], in_=rs[:, 0:n]), [b_rs], [b_rs])
                        op('dve', lambda e: e.scalar_tensor_tensor(out=qn[:, 0:n], in0=p1[:, 0:n], scalar=gcol[:, 0:1], in1=rs[:, 0:n], op0=ALU.mult, op1=ALU.mult),
                           [b_p1, b_gcol, b_rs], [b_qn])

                    def stC():
                        p3, b_p3 = ps_next()
                        op('pe', lambda e: e.matmul(p3[:, 0:n], lhsT=Rm, rhs=qn[:, 0:n], start=True, stop=True), [b_qn, b_cstb], [b_p3])
                        op('pool', lambda e: e.tensor_tensor(out=ra[:, 0:n], in0=qn[:, 0:n], in1=cosF[:, start:start + n], op=ALU.mult), [b_qn, b_cosF], [b_ra])
                        op('dve', lambda e: e.tensor_tensor(out=rb[:, 0:n], in0=p3[:, 0:n], in1=sinF[:, start:start + n], op=ALU.mult), [b_p3, b_sinF], [b_rb])
                        op('dve', lambda e: e.tensor_tensor(out=dstT[:, start - dst_off:start - dst_off + n], in0=ra[:, 0:n], in1=rb[:, 0:n], op=ALU.add), [b_ra, b_rb], [b_dst])
                    return [stA, stB, stC]

                def v_stage(start, n, bw):
                    hb_ = [b_hT[x] for x in range(start // 128, (start + n - 1) // 128 + 1)]

                    def stA():
                        p1, b_p1 = ps_next()
                        for kc in range(8):
                            op('pe', lambda e, kc=kc: e.matmul(p1[:, 0:n], lhsT=wd[:, kc, 256:384], rhs=hsl(kc, slice(start, start + n)), start=(kc == 0), stop=(kc == 7)),
                               hb_ + bw, [b_p1])
                        op('act', lambda e: e.copy(out=vTf[:, start:start + n], in_=p1[:, 0:n]), [b_p1], [b_vTf[start // 512]])
                    return [stA]

                def run_skewed(units):
                    nst = max(len(u_) for u_ in units)
                    for step in range(len(units) + nst - 1):
                        for j in range(nst):
                            i = step - j
                            if 0 <= i < len(units) and j < len(units[i]):
                                units[i][j]()

                for hp in range(4):
                    for g, d in enumerate((1, 4, 16)):
                        nb = 32 // d
                        nh = nb // 2
                        co = g * 512 + hp * 128
                        bw = load_w(wd[:, :, 0:128], w_in[:, C_QD + co:C_QD + co + 128], 8, 128, name='wdq')
                        bw += load_w(wd[:, :, 128:256], w_in[:, C_KD + co:C_KD + co + 128], 8, 128, name='wdk')
                        bw += load_w(wd[:, :, 256:384], w_in[:, C_VD + co:C_VD + co + 128], 8, 128, name='wdv')
                        cstart = TOK - 128 * d
                        kunits = []
                        s0 = cstart
                        while s0 < TOK:
                            n_ = min(512, TOK - s0)
                            kunits.append((s0, n_))
                            s0 += n_
                        units = []
                        for (s0, n_) in kunits:
                            units.append(qk_stages(128, gkc, b_gkc, s0, n_, kTf, 0, b_kTf[s0 // 512], bw))
                            units.append(v_stage(s0, n_, bw))
                        for sl in range(4):
                            units.append(v_stage(TOK + sl * 512, 512, bw))
                            units.append(qk_stages(128, gkc, b_gkc, TOK + sl * 512, 512, kTf, 0, b_kTf[4 + sl], bw))
                            units.append(qk_stages(0, gqc, b_gqc, TOK + sl * 512, 512, qTf, TOK, b_qTf[sl], bw))
                        run_skewed(units)
                        if DIL_STOP <= 2:
                            S.barrier()
                            return

                        def vidx(rho, n, nh=nh):
                            return rho * (nh + 1) + (n - (nh - 1))
                        vtiles = []
                        for rho in range(d):
                            for n in range(nh - 1, nb):
                                start = rho + d * 128 * n
                                vtiles.append((vidx(rho, n), start))
                        for i0 in range(0, len(vtiles), 8):
                            grp = vtiles[i0:i0 + 8]
                            pb, b_pb = psb_next()
                            for j, (vi, start) in enumerate(grp):
                                tsl = slice(start, start + 127 * d + 1, d)
                                vb_ = [b_vTf[x] for x in range(start // 512, (start + 127 * d) // 512 + 1)]
                                op('pe', lambda e, j=j, tsl=tsl: e.transpose(out=pb[:, j * 128:(j + 1) * 128], in_=vTf[:, tsl], identity=identb), vb_ + [b_cstb], [b_pb])
                            v0 = grp[0][0]
                            k_ = len(grp)
                            assert [g_[0] for g_ in grp] == list(range(v0, v0 + k_))
                            op('act', lambda e: e.copy(out=Vt[:, v0:v0 + k_, :, 0:64], in_=pb[:, 0:k_ * 128].rearrange("p (k h d) -> p k h d", k=k_, h=NH)),
                               [b_pb, b_Vones], [b_Vt[x] for x in range(v0, v0 + k_)])
                        if DIL_STOP <= 3:
                            S.barrier()
                            return
                        actr = 0
                        for rho in range(d):
                            for n in range(nh, nb):
                                pT, b_pT = pTr[actr % 3]
                                actr += 1
                                t0 = rho + d * 128 * (n - nh)
                                qsl = slice(t0, t0 + 127 * d + 1, d)
                                qb_ = [b_qTf[x] for x in range(t0 // 512, (t0 + 127 * d) // 512 + 1)]
                                pSh = [ps_next(), ps_next()]
                                for kt in range(2):
                                    ks = rho + d * 128 * (n - 1 + kt)
                                    ksl = slice(ks, ks + 127 * d + 1, d)
                                    kb_ = [b_kTf[x] for x in range(ks // 512, (ks + 127 * d) // 512 + 1)]
                                    for h in range(NH):
                                        op('pe', lambda e, kt=kt, h=h, ksl=ksl: e.matmul(pSh[h][0][:, kt * 128:(kt + 1) * 128], lhsT=kTf[h * 64:(h + 1) * 64, ksl],
                                                                                       rhs=qTf[h * 64:(h + 1) * 64, qsl], start=True, stop=True),
                                           kb_ + qb_, [pSh[h][1]])
                                for h in range(NH):
                                    op('act', lambda e, h=h: e.activation(out=pT[:, 2 * h:2 * h + 2, :].rearrange("p a q -> p (a q)"), in_=pSh[h][0][:, 0:256], func=AF.Exp, scale=0.125),
                                       [pSh[h][1]], [b_pT])
                                if DIL_STOP == 41:
                                    continue
                                op('pool' if actr % 2 else 'dve', lambda e: e.tensor_tensor(out=pT[:], in0=pT[:], in1=maskc[:], op=ALU.mult), [b_pT, b_maskc], [b_pT])
                                if n == nh:
                                    for h in range(NH):
                                        op('dve', lambda e, h=h: e.tensor_scalar(out=pT[:, 2 * h, :], in0=pT[:, 2 * h, :], scalar1=flag[:, 0:1], scalar2=None, op0=ALU.mult),
                                           [b_pT, b_flag], [b_pT])
                                if DIL_STOP == 42:
                                    continue
                                pO, b_pO = ps_next()
                                for h in range(NH):
                                    for kt in range(2):
                                        vi = vidx(rho, n - 1 + kt)
                                        op('pe', lambda e, h=h, kt=kt, vi=vi: e.matmul(pO[0:65, h * 128:(h + 1) * 128], lhsT=Vt[:, vi, h, :], rhs=pT[:, h * 2 + kt, :],
                                                                                      start=(kt == 0), stop=(kt == 1)),
                                           [b_Vt[vi], b_pT], [b_pO])
                                if DIL_STOP == 43:
                                    continue
                                av = acc[:, :, qsl]
                                pv_ = pO[0:65, 0:NH * 128].rearrange("p (h q) -> p h q", h=NH)
                                if g == 0:
                                    op('dve', lambda e: e.tensor_copy(out=av, in_=pv_), [b_pO], [b_acc])
                                else:
                                    op('dve', lambda e: e.tensor_tensor(out=av, in0=av, in1=pv_, op=ALU.add), [b_pO, b_acc], [b_acc])
                        if DIL_STOP <= 4 or DIL_STOP in (41, 42, 43):
                            S.barrier()
                            return
                    if DIL_STOP <= 5:
                        S.barrier()
                        return
                    for sl in range(4):
                        ts_ = slice(sl * 512, (sl + 1) * 512)
                        pNn, b_pNn = ps_next()
                        pDn, b_pDn = ps_next()
                        for h in range(NH):
                            sh = K_SH0 if h == 0 else K_SH1
                            op('pe', lambda e, h=h, sh=sh: e.matmul(pNn[:], lhsT=cst[0:64, sh:sh + 128], rhs=acc[0:64, h, ts_], start=(h == 0), stop=(h == 1)), [b_acc, b_cst], [b_pNn])
                        for h in range(NH):
                            op('pe', lambda e, h=h: e.matmul(pDn[:], lhsT=cst[64:65, K_BM:K_BM + 128] if h == 1 else cst[64:65, K_LO:K_LO + 128], rhs=acc[64:65, h, ts_],
                                                             start=(h == 0), stop=(h == 1)), [b_acc, b_cst], [b_pDn])
                        op('dve', lambda e: e.reciprocal(out=rec[:], in_=pDn[:]), [b_pDn], [b_rec])
                        op('dve', lambda e: e.tensor_tensor(out=ydT[:, hp, ts_], in0=pNn[:], in1=rec[:], op=ALU.mult), [b_pNn, b_rec], [b_ydT])
                S.barrier()
            if 'ydT' in dbg_out:
                tmpd, b_tmpd = sb('dbgy', [128, 2 * TOK])
                op('dve', lambda e: e.tensor_copy(out=tmpd[:, 0:TOK], in_=ydT[:, 0, :]), [b_ydT], [b_tmpd])
                op('dve', lambda e: e.tensor_copy(out=tmpd[:, TOK:], in_=ydT[:, 2, :]), [b_ydT, b_tmpd], [b_tmpd])
                dump('ydT', tmpd[:], b_tmpd)
            wud, _ = sb('wud', [128, 4, 1024], BF16)
            bwud = load_w(wud, w_up_dil, 4, 1024, name='wud')
            wg1, _ = sb('wg1', [128, 8, 1024], BF16)
            bwg1 = load_w(wg1, w_in[:, C_G + 1024:C_G + 2048], 8, 1024, name='wg1')
            sg, b_sg = sb('sg4', [128, 1024])
            tmpm, b_tmpm = sb('tmpm4', [128, 1024])
            mtile, b_mtile = None, None
            for t in range(NT):
                pU = []
                b_pU = []
                for nb_ in range(2):
                    p, b_p = ps_next()
                    for hp in range(4):
                        op('pe', lambda e, p=p, hp=hp, nb_=nb_, t=t: e.matmul(p[:], lhsT=ydT[:, hp, t * 128:(t + 1) * 128], rhs=wud[:, hp, nb_ * 512:(nb_ + 1) * 512],
                                                                          start=(hp == 0), stop=(hp == 3)), [b_ydT] + bwud, [b_p])
                    pU.append(p)
                    b_pU.append(b_p)
                gate_and_merge(t, pU, b_pU, wg1, bwg1, True, sg, b_sg, tmpm, b_tmpm, mtile, b_mtile)
            S.barrier()

    if upto >= 1:
        dts.close()
    if upto >= 4:
        phase_dil()
    ctxs.close()
    if upto >= 3:
        phase_ssd_final()

    if upto >= 5:
        with ExitStack() as ph:
            sb = mk_sb(ph)
            memT, b_memT = sb('memT', [128, 8, 256], BF16)
            kTm, b_kTm = sb('kTm', [128, 12, 256], BF16)
            vtok, b_vtok = sb('vtok', [128, 2, 1536], BF16)
            gqm, b_gqm = bcast_load(sb, 'gqm', mem_q_norm, 384)
            gkm, b_gkm = bcast_load(sb, 'gkm', mem_k_norm, 384)
            qf, b_qf = sb('qf', [128, 1536])
            sq, b_sq = sb('sq5', [128, 1536])
            qb, b_qb = sb('qb', [128, 1536], BF16)
            st, b_st = sb('st5', [128, 8])
            with ExitStack() as ph2:
                sb2 = mk_sb(ph2)
                gmem, b_gmem = bcast_load(sb2, 'gmem', mem_norm, 1024)
                xm, b_xm = sb2('xm', [128, 1024])
                hm, b_hm = sb2('hm', [128, 1024], BF16)
                wkv, _ = sb2('wkv', [128, 8, 512], BF16)
                ktok, b_ktok = sb2('ktok', [128, 2, 1536])
                for mt in range(2):
                    S.dma('sp', xm[:], mem_d[mt * 128:(mt + 1) * 128, :], [], [b_xm], b_xm)
                    op('act', lambda e: e.activation(out=sq[:, 0:1024], in_=xm[:], func=AF.Square, accum_out=st[:, 0:1]), [b_xm], [b_sq, b_st])
                    rstd_from_ssq(st[:, 0:1], st[:, 1:2], 1024, b_st)
                    op('dve', lambda e: e.scalar_tensor_tensor(out=hm[:], in0=xm[:], scalar=st[:, 1:2], in1=gmem[:], op0=ALU.mult, op1=ALU.mult),
                       [b_xm, b_st, b_gmem], [b_hm])
                    pb, b_pb = psb_next()
                    for kc in range(8):
                        op('pe', lambda e, kc=kc, pb=pb: e.transpose(out=pb[:, kc * 128:(kc + 1) * 128], in_=hm[:, kc * 128:(kc + 1) * 128], identity=identb),
                           [b_hm, b_cstb], [b_pb])
                    op('act', lambda e, mt=mt, pb=pb: e.copy(out=memT[:, :, mt * 128:(mt + 1) * 128], in_=pb[:].rearrange("p (k t) -> p k t", k=8)), [b_pb], [b_memT])
                for nb in range(6):
                    bw = load_w(wkv, w_mem_kv[:, nb * 512:(nb + 1) * 512], 8, 512, name='wkv')
                    for mt in range(2):
                        p, b_p = ps_next()
                        for kc in range(8):
                            op('pe', lambda e, p=p, kc=kc, mt=mt: e.matmul(p[:], lhsT=memT[:, kc, mt * 128:(mt + 1) * 128], rhs=wkv[:, kc, :], start=(kc == 0), stop=(kc == 7)),
                               [b_memT] + bw, [b_p])
                        if nb < 3:
                            op('act', lambda e, p=p, mt=mt, nb=nb: e.copy(out=ktok[:, mt, nb * 512:(nb + 1) * 512], in_=p[:]), [b_p], [b_ktok])
                        else:
                            op('act', lambda e, p=p, mt=mt, nb=nb: e.copy(out=vtok[:, mt, (nb - 3) * 512:(nb - 2) * 512], in_=p[:]), [b_p], [b_vtok])
                for mt in range(2):
                    op('act', lambda e, mt=mt: e.activation(out=sq[:], in_=ktok[:, mt, :], func=AF.Square), [b_ktok], [b_sq])
                    op('dve', lambda e: e.tensor_reduce(out=st[:, 0:4], in_=sq[:].rearrange("p (h d) -> p h d", h=4), axis=AX.X, op=ALU.add), [b_sq], [b_st])
                    rstd_from_ssq(st[:, 0:4], st[:, 4:8], 384, b_st)
                    op('dve', lambda e, mt=mt: e.tensor_tensor(out=qf[:].rearrange("p (h d) -> p h d", h=4), in0=ktok[:, mt, :].rearrange("p (h d) -> p h d", h=4),
                                                               in1=st[:, 4:8].unsqueeze(2).to_broadcast([128, 4, 384]), op=ALU.mult), [b_ktok, b_st], [b_qf])
                    op('pool', lambda e: e.tensor_tensor(out=qb[:].rearrange("p (h d) -> p h d", h=4), in0=qf[:].rearrange("p (h d) -> p h d", h=4),
                                                         in1=gkm[:].unsqueeze(1).to_broadcast([128, 4, 384]), op=ALU.mult), [b_qf, b_gkm], [b_qb])
                    for half, cnt in ((0, 8), (1, 4)):
                        pb, b_pb = psb_next()
                        for k in range(cnt):
                            kc = half * 8 + k
                            op('pe', lambda e, pb=pb, k=k, kc=kc: e.transpose(out=pb[:, k * 128:(k + 1) * 128], in_=qb[:, kc * 128:(kc + 1) * 128], identity=identb),
                               [b_qb, b_cstb], [b_pb])
                        op('act', lambda e, pb=pb, half=half, cnt=cnt, mt=mt: e.copy(out=kTm[:, half * 8:half * 8 + cnt, mt * 128:(mt + 1) * 128],
                                                                                 in_=pb[:, 0:cnt * 128].rearrange("p (k t) -> p k t", k=cnt)), [b_pb], [b_kTm])
                S.barrier()
            wqm, _ = sb('wqm', [128, 8, 1536], BF16)
            bwqm = load_w(wqm, w_in[:, C_QM:C_QM + 1536], 8, 1536, name='wqm')
            wum, _ = sb('wum', [128, 12, 1024], BF16)
            bwum = load_w(wum, w_up_mem, 12, 1024, name='wum')
            wg2, _ = sb('wg2', [128, 8, 1024], BF16)
            bwg2 = load_w(wg2, w_in[:, C_G + 2048:C_G + 3072], 8, 1024, name='wg2')
            qmT, b_qmT = sb('qmT', [128, 12, 128], BF16)
            pTm = [sb('pTm%d' % i, [128, 512], BF16) for i in range(2)]
            recm, b_recm = sb('recm', [128, 128])
            ymT, b_ymT = sb('ymT', [128, 12, 128], BF16)
            sg, b_sg = sb('sg5', [128, 1024])
            tmpm, b_tmpm = sb('tmpm5', [128, 1024])
            mtile, b_mtile = sb('mtile5', [128, 1024])
            onesb = cstb[:, K_ONES:K_ONES + 128]
            for t in range(NT):
                for nb in range(3):
                    p, b_p = ps_next()
                    for kc in range(8):
                        op('pe', lambda e, p=p, kc=kc, nb=nb, t=t: e.matmul(p[:], lhsT=hsl(kc, own(t)), rhs=wqm[:, kc, nb * 512:(nb + 1) * 512], start=(kc == 0), stop=(kc == 7)),
                           [b_hT[16 + t]] + bwqm, [b_p])
                    op('act', lambda e, p=p, nb=nb: e.copy(out=qf[:, nb * 512:(nb + 1) * 512], in_=p[:]), [b_p], [b_qf])
                op('act', lambda e: e.activation(out=sq[:], in_=qf[:], func=AF.Square), [b_qf], [b_sq])
                op('dve', lambda e: e.tensor_reduce(out=st[:, 0:4], in_=sq[:].rearrange("p (h d) -> p h d", h=4), axis=AX.X, op=ALU.add), [b_sq], [b_st])
                rstd_from_ssq(st[:, 0:4], st[:, 4:8], 384, b_st)
                op('dve', lambda e: e.tensor_tensor(out=qf[:].rearrange("p (h d) -> p h d", h=4), in0=qf[:].rearrange("p (h d) -> p h d", h=4),
                                                    in1=st[:, 4:8].unsqueeze(2).to_broadcast([128, 4, 384]), op=ALU.mult), [b_qf, b_st], [b_qf])
                op('pool', lambda e: e.tensor_tensor(out=qb[:].rearrange("p (h d) -> p h d", h=4), in0=qf[:].rearrange("p (h d) -> p h d", h=4),
                                                     in1=gqm[:].unsqueeze(1).to_broadcast([128, 4, 384]), op=ALU.mult), [b_qf, b_gqm], [b_qb])
                for half, cnt in ((0, 8), (1, 4)):
                    pb, b_pb = psb_next()
                    for k in range(cnt):
                        kc = half * 8 + k
                        op('pe', lambda e, pb=pb, k=k, kc=kc: e.transpose(out=pb[:, k * 128:(k + 1) * 128], in_=qb[:, kc * 128:(kc + 1) * 128], identity=identb),
                           [b_qb, b_cstb], [b_pb])
                    op('act', lambda e, pb=pb, half=half, cnt=cnt: e.copy(out=qmT[:, half * 8:half * 8 + cnt, :], in_=pb[:, 0:cnt * 128].rearrange("p (k t) -> p k t", k=cnt)),
                       [b_pb], [b_qmT])
                for mt in range(2):
                    pS, b_pS = ps_next()
                    for h in range(4):
                        for j in range(3):
                            op('pe', lambda e, pS=pS, h=h, j=j, mt=mt: e.matmul(pS[:, h * 128:(h + 1) * 128], lhsT=kTm[:, h * 3 + j, mt * 128:(mt + 1) * 128], rhs=qmT[:, h * 3 + j, :],
                                                                             start=(j == 0), stop=(j == 2)), [b_kTm, b_qmT], [b_pS])
                    op('act', lambda e, pS=pS, mt=mt: e.activation(out=pTm[mt][0][:], in_=pS[:], func=AF.Exp, scale=1.0 / math.sqrt(384.0)), [b_pS], [pTm[mt][1]])
                for h in range(4):
                    pN, b_pN = ps_next()
                    for j in range(4):
                        for mt in range(2):
                            lhs = vtok[:, mt, h * 384 + j * 128:h * 384 + (j + 1) * 128] if j < 3 else onesb
                            op('pe', lambda e, pN=pN, j=j, mt=mt, h=h, lhs=lhs: e.matmul(pN[:, j * 128:(j + 1) * 128], lhsT=lhs, rhs=pTm[mt][0][:, h * 128:(h + 1) * 128],
                                                                                      start=(mt == 0), stop=(mt == 1)), [b_vtok, b_cstb, pTm[mt][1]], [b_pN])
                    op('dve', lambda e, pN=pN: e.reciprocal(out=recm[:], in_=pN[:, 384:512]), [b_pN], [b_recm])
                    op('dve', lambda e, pN=pN, h=h: e.tensor_tensor(out=ymT[:, h * 3:(h + 1) * 3, :], in0=pN[:, 0:384].rearrange("p (j q) -> p j q", j=3),
                                                                    in1=recm[:].unsqueeze(1).to_broadcast([128, 3, 128]), op=ALU.mult), [b_pN, b_recm], [b_ymT])
                if t == 0 and 'ymT' in dbg_out:
                    tmpd, b_tmpd = sb('dbgm', [128, 1536])
                    op('dve', lambda e: e.tensor_copy(out=tmpd[:], in_=ymT[:].rearrange("p k t -> p (k t)")), [b_ymT], [b_tmpd])
                    dump('ymT', tmpd[:], b_tmpd)
                pU = []
                b_pU = []
                for nb in range(2):
                    p, b_p = ps_next()
                    for kc in range(12):
                        op('pe', lambda e, p=p, kc=kc, nb=nb: e.matmul(p[:], lhsT=ymT[:, kc, :], rhs=wum[:, kc, nb * 512:(nb + 1) * 512], start=(kc == 0), stop=(kc == 11)),
                           [b_ymT] + bwum, [b_p])
                    pU.append(p)
                    b_pU.append(b_p)
                gate_and_merge(t, pU, b_pU, wg2, bwg2, False, sg, b_sg, tmpm, b_tmpm, mtile, b_mtile)
            S.barrier()
    S.barrier()
    mid.close()

    if upto >= 6:
        late = ExitStack()
        sbl = mk_sb(late)
        rstd2, b_rstd2 = sbl('rstd2', [128, NT])
        gffn, b_gffn = bcast_load(sbl, 'gffn', norm_ffn, 1024)
        eidx, _ = sbl('eidx', [128, NT, 128], U32)
        gate, _ = sbl('gate', [128, NT, 128])
        b_eidx = [Buf('eidx%d' % t) for t in range(NT)]
        b_gate = [Buf('gate%d' % t) for t in range(NT)]
        keysT, b_keysT = sbl('keysT', [128, 256])
        qsc = nc.dram_tensor('qsc', [8, 128, 4096], F32, kind="Internal").ap()
        b_qsc = [Buf('qsc%d' % i) for i in range(8)]
        with ExitStack() as ph:
            sb = mk_sb(ph)
            h2T, _ = sb('h2T', [128, 8, TOK], BF16)
            b_h2T = [Buf('h2T%d' % t) for t in range(NT)]
            with ExitStack() as pha:
                sb = mk_sb(pha)
                wo, _ = sb('wo', [128, 8, 1024], BF16)
                bwo = load_w(wo, w_out, 8, 1024, name='wo')
                mbr = [sb('mb%d' % i, [128, 1024], BF16) for i in range(2)]
                mTr = [sb('mT%d' % i, [128, 8, 128], BF16) for i in range(2)]
                xts = [sb('xo%d' % i, [128, 1024]) for i in range(3)]
                mts = [sb('mo%d' % i, [128, 1024]) for i in range(2)]
                x1s = [sb('x1o%d' % i, [128, 1024]) for i in range(2)]
                sq, b_sq = sb('sq6', [128, 1024])
                str6 = [sb('st6_%d' % i, [128, 2]) for i in range(2)]
                hbr6 = [sb('hb6_%d' % i, [128, 1024], BF16) for i in range(2)]
                live = {}

                def stA(t):
                    xt, b_xt = xts[t % 3]
                    mtl, b_mtl = mts[t % 2]
                    mb_, b_mb = mbr[t % 2]
                    S.dma('sp', xt[:], xa[TOK + t * 128:TOK + (t + 1) * 128, :], [], [b_xt], b_xt)
                    S.dma('sp', mtl[:], msc[t * 128:(t + 1) * 128, :], [b_msc[t]], [b_mtl], b_mtl)
                    op('dve', lambda e: e.tensor_copy(out=mb_[:], in_=mtl[:]), [b_mtl], [b_mb])
                    pb, b_pb = psb_next()
                    for kc in range(8):
                        op('pe', lambda e, kc=kc: e.transpose(out=pb[:, kc * 128:(kc + 1) * 128], in_=mb_[:, kc * 128:(kc + 1) * 128], identity=identb), [b_mb, b_cstb], [b_pb])
                    live[('A', t)] = (pb, b_pb)

                def stB(t):
                    pb, b_pb = live.pop(('A', t))
                    mT, b_mT = mTr[t % 2]
                    op('act', lambda e: e.copy(out=mT[:], in_=pb[:].rearrange("p (k t) -> p k t", k=8)), [b_pb], [b_mT])
                    ps_ = []
                    for nb in range(2):
                        p, b_p = ps_next()
                        for kc in range(8):
                            op('pe', lambda e, kc=kc, nb=nb: e.matmul(p[:], lhsT=mT[:, kc, :], rhs=wo[:, kc, nb * 512:(nb + 1) * 512], start=(kc == 0), stop=(kc == 7)),
                               [b_mT] + bwo, [b_p])
                        ps_.append((p, b_p))
                    live[('B', t)] = ps_

                def stC(t):
                    ps_ = live.pop(('B', t))
                    xt, b_xt = xts[t % 3]
                    x1t, b_x1t = x1s[t % 2]
                    st, b_st = str6[t % 2]
                    hb, b_hb = hbr6[t % 2]
                    for nb in range(2):
                        p, b_p = ps_[nb]
                        op('dve', lambda e, nb=nb: e.tensor_tensor(out=x1t[:, nb * 512:(nb + 1) * 512], in0=p[:], in1=xt[:, nb * 512:(nb + 1) * 512], op=ALU.add),
                           [b_p, b_xt], [b_x1t])
                    S.dma('sp', out_d[t * 128:(t + 1) * 128, :], x1t[:], [b_x1t], [b_x1d[t]], b_x1t)
                    if t == 0:
                        dump('x1', x1t[:], b_x1t)
                    op('act', lambda e: e.activation(out=sq[:], in_=x1t[:], func=AF.Square, accum_out=st[:, 0:1]), [b_x1t], [b_sq, b_st])
                    op('act', lambda e: e.activation(out=rstd2[:, t:t + 1], in_=st[:, 0:1], func=AF.Sqrt, scale=1.0 / 1024, bias=epst[:, 0:1]), [b_st, b_eps], [b_rstd2])
                    op('dve', lambda e: e.reciprocal(out=rstd2[:, t:t + 1], in_=rstd2[:, t:t + 1]), [b_rstd2], [b_rstd2])
                    op('dve', lambda e: e.scalar_tensor_tensor(out=hb[:], in0=x1t[:], scalar=rstd2[:, t:t + 1], in1=gffn[:], op0=ALU.mult, op1=ALU.mult),
                       [b_x1t, b_rstd2, b_gffn], [b_hb])
                    pb, b_pb = psb_next()
                    for kc in range(8):
                        op('pe', lambda e, kc=kc: e.transpose(out=pb[:, kc * 128:(kc + 1) * 128], in_=hb[:, kc * 128:(kc + 1) * 128], identity=identb), [b_hb, b_cstb], [b_pb])
                    live[('C', t)] = (pb, b_pb)

                def stD(t):
                    pb, b_pb = live.pop(('C', t))
                    op('act', lambda e: e.copy(out=h2T[:, :, t * 128:(t + 1) * 128], in_=pb[:].rearrange("p (k t) -> p k t", k=8)), [b_pb], [b_h2T[t]])
                stages = (stA, stB, stC, stD)
                for step in range(NT + 3):
                    for j in (3, 2, 1, 0):
                        t = step - j
                        if 0 <= t < NT:
                            stages[j](t)
                S.barrier()
            sb = mk_sb(ph)
            wpq, _ = sb('wpq', [128, 8, 2048], BF16)
            bwpq = load_w(wpq, peer_w_q, 8, 2048, name='wpq')
            kr_, b_kr_ = sb('keysr', [128, 256])
            S.dma('sp', kr_[:, 0:128], peer_keys1, [], [b_kr_], b_kr_)
            S.dma('sp', kr_[:, 128:256], peer_keys2, [b_kr_], [b_kr_], b_kr_)
            p, b_p = ps_next()
            for i in range(2):
                op('pe', lambda e, p=p, i=i: e.transpose(out=p[:, i * 128:(i + 1) * 128], in_=kr_[:, i * 128:(i + 1) * 128], identity=identf), [b_kr_, b_cst], [b_p])
            op('act', lambda e, p=p: e.copy(out=keysT[:], in_=p[:, 0:256]), [b_p], [b_keysT])
            qTsr = [sb('qTs%d' % i, [128, 16, 256]) for i in range(2)]
            for sl in range(8):
                qTs, b_qTs = qTsr[sl % 2]
                for c in range(16):
                    p, b_p = ps_next()
                    for kc in range(8):
                        op('pe', lambda e, p=p, kc=kc, c=c, sl=sl: e.matmul(p[:, 0:256], lhsT=wpq[:, kc, c * 128:(c + 1) * 128], rhs=h2T[:, kc, sl * 256:(sl + 1) * 256],
                                                                         start=(kc == 0), stop=(kc == 7)), b_h2T[2 * sl:2 * sl + 2] + bwpq, [b_p])
                    op('act', lambda e, p=p, c=c, qTs=qTs: e.copy(out=qTs[:, c, :], in_=p[:, 0:256]), [b_p], [b_qTs])
                S.dma('sp', qsc[sl], qTs[:].rearrange("p c k -> p (c k)"), [b_qTs], [b_qsc[sl]], b_qTs)
            S.barrier()

        if upto >= 7:
            with ExitStack() as ph:
                sb = mk_sb(ph)
                NU = 16
                GS = 8
                uvs = [sb('uv%d' % i, [128, 2048], BF16) for i in range(NU)]
                dgs = [sb('dg%d' % i, [128, 128], BF16) for i in range(4)]
                h2f, b_h2f = sb('h2f', [128, 1024], BF16)
                junkr = [sb('junk%d' % i, [128, 1024], BF16) for i in range(4)]
                dotr = [sb('dots%d' % i, [128, GS]) for i in range(2)]
                wgr = [sb('wgt%d' % i, [128, GS]) for i in range(2)]
                ots = [sb('ot%d' % i, [128, 1024]) for i in range(2)]
                x1p = [sb('x1p%d' % i, [128, 1024]) for i in range(2)]
                qTtr = [sb('qTt%d' % i, [128, 16, 128]) for i in range(2)]
                scs, b_scs = sb('scs', [128, 16, 128])
                sc2, b_sc2 = sb('sc2', [128, 16, 128])
                vals, b_vals = sb('vals', [128, 16, 16])
                idxs, b_idxs = sb('idxs', [128, 16, 16], U32)
                idxf, b_idxf = sb('idxf', [128, 16, 16])
                cand, b_cand = sb('cand', [128, 8, 256])
                cand2, b_cand2 = sb('cand2', [128, 8, 256])
                scv, b_scv = sb('scv', [128, 8, 16])
                ci, b_ci = sb('ci', [128, 8, 16], U32)
                cia, b_cia = sb('cia', [128, 8, 16], U32)
                cib, b_cib = sb('cib', [128, 8, 16], U32)
                fa, b_fa = sb('fa', [128, 8, 16])
                fb, b_fb = sb('fb', [128, 8, 16])
                eq, b_eq = sb('eq', [128, 8, 16, 16])
                e1, b_e1 = sb('e1', [128, 8, 16])
                e2, b_e2 = sb('e2', [128, 8, 16])
                nm, b_nm = sb('nm', [128, 8])
                sm, b_sm = sb('sm', [128, 8])
                ex, b_ex = sb('ex', [128, 8, 16])
                iota16 = cst[:, K_IOTA:K_IOTA + 16]

                def route_gen(t):
                    qTt, b_qTt = qTtr[t % 2]
                    S.dma('sp', qTt[:], qsc[t // 2].rearrange("p (c k) -> p c k", c=16)[:, :, (t % 2) * 128:(t % 2 + 1) * 128], [b_qsc[t // 2]], [b_qTt], b_qTt)
                    yield
                    for q4 in range(4):
                        p, b_p = ps_next()
                        for cc in range(4):
                            c = q4 * 4 + cc
                            op('pe', lambda e, p=p, c=c, cc=cc: e.matmul(p[:, cc * 128:(cc + 1) * 128], lhsT=qTt[:, c, :],
                                                                       rhs=keysT[:, (c % 2) * 128:(c % 2 + 1) * 128], start=True, stop=True), [b_qTt, b_keysT], [b_p])
                        op('act', lambda e, p=p, q4=q4: e.copy(out=scs[:, q4 * 4:(q4 + 1) * 4, :], in_=p[:].rearrange("p (c k) -> p c k", c=4)), [b_p], [b_scs])
                        yield
                    bv = [Buf('vals%d' % c) for c in range(16)]
                    bi = [Buf('idxs%d' % c) for c in range(16)]
                    b2 = [Buf('sc2_%d' % c) for c in range(16)]
                    for c in range(16):
                        op('dve', lambda e, c=c: e.max(out=vals[:, c, 0:8], in_=scs[:, c, :]), [b_scs, b_vals], [bv[c]])
                        if c % 4 == 3:
                            yield
                    for c in range(16):
                        op('dve', lambda e, c=c: e.max_index(out=idxs[:, c, 0:8], in_max=vals[:, c, 0:8], in_values=scs[:, c, :]), [b_scs, bv[c], b_idxs], [bi[c]])
                        if c % 4 == 3:
                            yield
                    for c in range(16):
                        op('dve', lambda e, c=c: e.match_replace(out=sc2[:, c, :], in_to_replace=vals[:, c, 0:8], in_values=scs[:, c, :], imm_value=-1e30), [b_scs, bv[c], b_sc2], [b2[c]])
                        if c % 4 == 3:
                            yield
                    for c in range(16):
                        op('dve', lambda e, c=c: e.max(out=vals[:, c, 8:16], in_=sc2[:, c, :]), [b2[c]], [bv[c]])
                        if c % 4 == 3:
                            yield
                    for c in range(16):
                        op('dve', lambda e, c=c: e.max_index(out=idxs[:, c, 8:16], in_max=vals[:, c, 8:16], in_values=sc2[:, c, :]), [b2[c], bv[c]], [bi[c]])
                        if c % 4 == 3:
                            yield
                    op('dve', lambda e: e.tensor_copy(out=idxf[:], in_=idxs[:]), bi, [b_idxf, b_idxs])
                    v4 = vals[:].rearrange("p (h two) k -> p h two k", two=2)
                    op('dve', lambda e, v4=v4: e.tensor_tensor(out=cand[:].rearrange("p h (a b) -> p h a b", a=16), in0=v4[:, :, 0, :].unsqueeze(3).to_broadcast([128, 8, 16, 16]),
                                                               in1=v4[:, :, 1, :].unsqueeze(2).to_broadcast([128, 8, 16, 16]), op=ALU.add), bv + b2, [b_cand, b_vals, b_sc2])
                    bs_ = [Buf('scv%d' % h) for h in range(8)]
                    bc_ = [Buf('ci%d' % h) for h in range(8)]
                    b3 = [Buf('cand2_%d' % h) for h in range(8)]
                    for h in range(8):
                        op('dve', lambda e, h=h: e.max(out=scv[:, h, 0:8], in_=cand[:, h, :]), [b_cand, b_scv], [bs_[h]])
                        if h % 4 == 3:
                            yield
                    for h in range(8):
                        op('dve', lambda e, h=h: e.max_index(out=ci[:, h, 0:8], in_max=scv[:, h, 0:8], in_values=cand[:, h, :]), [b_cand, bs_[h], b_ci], [bc_[h]])
                        if h % 4 == 3:
                            yield
                    for h in range(8):
                        op('dve', lambda e, h=h: e.match_replace(out=cand2[:, h, :], in_to_replace=scv[:, h, 0:8], in_values=cand[:, h, :], imm_value=-1e30), [b_cand, bs_[h], b_cand2], [b3[h]])
                        if h % 4 == 3:
                            yield
                    for h in range(8):
                        op('dve', lambda e, h=h: e.max(out=scv[:, h, 8:16], in_=cand2[:, h, :]), [b3[h]], [bs_[h]])
                        if h % 4 == 3:
                            yield
                    for h in range(8):
                        op('dve', lambda e, h=h: e.max_index(out=ci[:, h, 8:16], in_max=scv[:, h, 8:16], in_values=cand2[:, h, :]), [b3[h], bs_[h]], [bc_[h]])
                        if h % 4 == 3:
                            yield
                    yield
                    op('dve', lambda e: e.tensor_copy(out=cand2[:, 0, 0:1], in_=cand2[:, 0, 0:1]), bs_ + bc_ + b3, [b_scv, b_ci, b_cand2])
                    op('dve', lambda e: e.tensor_scalar(out=cia[:], in0=ci[:], scalar1=4, scalar2=None, op0=ALU.arith_shift_right), [b_ci], [b_cia])
                    op('dve', lambda e: e.tensor_scalar(out=cib[:], in0=ci[:], scalar1=15, scalar2=None, op0=ALU.bitwise_and), [b_ci], [b_cib])
                    op('dve', lambda e: e.tensor_copy(out=fa[:], in_=cia[:]), [b_cia], [b_fa])
                    op('dve', lambda e: e.tensor_copy(out=fb[:], in_=cib[:]), [b_cib], [b_fb])
                    yield
                    i4 = idxf[:].rearrange("p (h two) k -> p h two k", two=2)
                    iob = iota16.unsqueeze(1).unsqueeze(1).to_broadcast([128, 8, 16, 16])
                    for (fsel, which, eo, b_eo) in ((fa, 0, e1, b_e1), (fb, 1, e2, b_e2)):
                        fbuf = b_fa if which == 0 else b_fb
                        op('dve', lambda e, fsel=fsel: e.tensor_tensor(out=eq[:], in0=fsel[:].unsqueeze(3).to_broadcast([128, 8, 16, 16]), in1=iob, op=ALU.is_equal),
                           [fbuf, b_cst], [b_eq])
                        op('dve', lambda e, which=which, i4=i4: e.tensor_tensor(out=eq[:], in0=eq[:], in1=i4[:, :, which, :].unsqueeze(2).to_broadcast([128, 8, 16, 16]), op=ALU.mult),
                           [b_eq, b_idxf], [b_eq])
                        op('dve', lambda e, eo=eo: e.tensor_reduce(out=eo[:], in_=eq[:], axis=AX.X, op=ALU.add), [b_eq], [b_eo])
                        yield
                    op('dve', lambda e: e.scalar_tensor_tensor(out=e1[:], in0=e1[:], scalar=128.0, in1=e2[:], op0=ALU.mult, op1=ALU.add), [b_e1, b_e2], [b_e1])
                    op('dve', lambda e, t=t: e.tensor_copy(out=eidx[:, t, :], in_=e1[:].rearrange("p h k -> p (h k)")), [b_e1], [b_eidx[t]])
                    op('dve', lambda e: e.tensor_scalar(out=nm[:], in0=scv[:, :, 0], scalar1=-1.0, scalar2=None, op0=ALU.mult), [b_scv], [b_nm])
                    for h in range(8):
                        op('act', lambda e, h=h: e.activation(out=ex[:, h, :], in_=scv[:, h, :], func=AF.Exp, bias=nm[:, h:h + 1], accum_out=sm[:, h:h + 1]),
                           [b_scv, b_nm], [b_ex, b_sm])
                        if h % 4 == 3:
                            yield
                    op('dve', lambda e: e.reciprocal(out=sm[:], in_=sm[:]), [b_sm], [b_sm])
                    op('dve', lambda e, t=t: e.tensor_tensor(out=gate[:, t, :].rearrange("p (h k) -> p h k", h=8), in0=ex[:], in1=sm[:].unsqueeze(2).to_broadcast([128, 8, 16]), op=ALU.mult),
                       [b_ex, b_sm], [b_gate[t]])
                    yield

                rr['n'] = 4
                for _ in route_gen(0):
                    pass
                for t in range(NT):
                    rg = route_gen(t + 1) if t + 1 < NT else iter(())
                    x1t, b_x1t = x1p[t % 2]
                    S.dma('sp', x1t[:], out_d[t * 128:(t + 1) * 128, :], [b_x1d[t]], [b_x1t], b_x1t)
                    op('dve', lambda e, t=t, x1t=x1t: e.scalar_tensor_tensor(out=h2f[:], in0=x1t[:], scalar=rstd2[:, t:t + 1], in1=gffn[:], op0=ALU.mult, op1=ALU.mult),
                       [b_x1t, b_rstd2, b_gffn], [b_h2f])
                    pA = [psf[4], psf[5]]
                    for sg_ in range(128 // GS):
                        dots, b_dots = dotr[sg_ % 2]
                        wgt, b_wgt = wgr[sg_ % 2]
                        for k in range(GS):
                            s = sg_ * GS + k
                            uv, b_uv = uvs[s % NU]
                            S.dma('pool', None, None, [b_eidx[t]], [b_uv], b_uv,
                                  indirect=lambda e, uv=uv, t=t, s=s: e.indirect_dma_start(out=uv[:], out_offset=None, in_=cb16,
                                                                                       in_offset=bass.IndirectOffsetOnAxis(ap=eidx[:, t, s:s + 1], axis=0)))
                            junk, b_junk = junkr[s % 4]
                            op('dve', lambda e, uv=uv, k=k, dots=dots, junk=junk: e.scalar_tensor_tensor(out=junk[:], in0=uv[:, 0:1024], scalar=1.0, in1=h2f[:], op0=ALU.mult, op1=ALU.mult,
                                                                                            accum_out=dots[:, k:k + 1]),
                               [b_uv, b_h2f], [b_junk] + ([b_dots] if k in (0, GS - 1) else []))
                        op('act', lambda e, dots=dots, wgt=wgt: e.activation(out=wgt[:], in_=dots[:], func=AF.Gelu), [b_dots], [b_wgt])
                        for _ in range(3):
                            next(rg, None)
                        op('dve', lambda e, t=t, wgt=wgt, sg_=sg_: e.tensor_tensor(out=wgt[:], in0=wgt[:], in1=gate[:, t, sg_ * GS:(sg_ + 1) * GS], op=ALU.mult), [b_wgt, b_gate[t]], [b_wgt])
                        for k in range(GS):
                            s = sg_ * GS + k
                            uv, b_uv = uvs[s % NU]
                            dg, b_dg = dgs[s % 4]
                            op('act', lambda e, dg=dg, k=k, wgt=wgt: e.activation(out=dg[:], in_=identf, func=AF.Copy, scale=wgt[:, k:k + 1]), [b_cst, b_wgt], [b_dg])
                            for nb in range(2):
                                op('pe', lambda e, nb=nb, dg=dg, uv=uv, s=s, pA=pA: e.matmul(pA[nb][0][:], lhsT=dg[:], rhs=uv[:, 1024 + nb * 512:1024 + (nb + 1) * 512],
                                                                                         start=(s == 0), stop=(s == 127)),
                                   [b_dg, b_uv], [pA[nb][1]])
                    for _ in rg:
                        pass
                    ot, b_ot = ots[t % 2]
                    for nb in range(2):
                        op('dve', lambda e, nb=nb, ot=ot, x1t=x1t, pA=pA: e.tensor_tensor(out=ot[:, nb * 512:(nb + 1) * 512], in0=pA[nb][0][:], in1=x1t[:, nb * 512:(nb + 1) * 512], op=ALU.add),
                           [pA[nb][1], b_x1t], [b_ot])
                    S.dma('sp', out_d[t * 128:(t + 1) * 128, :], ot[:], [b_ot], [b_x1d[t]], b_ot)
                rr['n'] = 6
                S.barrier()
        late.close()
    S.finish()
    top.close()
    return nc, S


_CST = None


def _consts():
    global _CST
    if _CST is None:
        c = np.zeros((128, K_END), np.float32)
        p = np.arange(128)[:, None]
        f = np.arange(128)[None, :]
        c[:, K_ID:K_ID + 128] = (p == f)
        c[:, K_TRIU:K_TRIU + 128] = (p <= f)
        c[:, K_TRIL:K_TRIL + 128] = (p >= f)
        c[:, K_TRILS:K_TRILS + 128] = (p > f)
        c[:, K_ONES:K_ONES + 128] = 1.0
        half = 32
        inv = (np.float32(10000.0) ** (-np.arange(half, dtype=np.float32) / np.float32(half))).astype(np.float32)
        c[:, K_INVF:K_INVF + 32] = inv[None, :]
        c[:, K_BM:K_BM + 128] = (p // 64 == f // 64)
        rm = np.zeros((128, 128), np.float32)
        for cc in range(128):
            if cc % 64 < 32:
                rm[cc + 32, cc] = -1.0
            else:
                rm[cc - 32, cc] = 1.0
        c[:, K_RM:K_RM + 128] = rm
        c[0:64, K_SH0:K_SH0 + 64] = np.eye(64, dtype=np.float32)
        c[0:64, K_SH1 + 64:K_SH1 + 128] = np.eye(64, dtype=np.float32)
        c[:, K_LO:K_LO + 64] = 1.0
        c[:, K_INVFC] = inv[np.arange(128) % 32]
        c[:, K_IOTA:K_IOTA + 16] = np.arange(16, dtype=np.float32)[None, :]
        _CST = c
    return _CST


def make_in_maps(inputs, cores=range(8)):
    x = np.asarray(inputs['x'], np.float32)
    pos = np.asarray(inputs['positions'], np.int32)
    mem = np.asarray(inputs['mem'], np.float32)
    shared = {}
    for k in ('norm_mix', 'conv_b', 'dt_bias', 'a_log', 'd_skip', 'ssd_norm', 'dil_q_norm', 'dil_k_norm', 'mem_norm', 'mem_q_norm', 'mem_k_norm', 'norm_ffn'):
        shared[k] = np.ascontiguousarray(np.asarray(inputs[k], np.float32).reshape(1, -1))
    for k in ('w_in', 'conv_w', 'w_mem_kv', 'w_up_ssd', 'w_up_dil', 'w_up_mem', 'w_out', 'peer_w_q', 'peer_keys1', 'peer_keys2', 'peer_u', 'peer_v'):
        shared[k] = np.ascontiguousarray(np.asarray(inputs[k], np.float32)[0])
    shared['cst'] = _consts()
    maps = []
    for c in cores:
        b, hf = c // 2, c % 2
        m = dict(shared)
        if hf == 0:
            xa = np.concatenate([np.zeros((TOK, 1024), np.float32), x[b, :TOK]], 0)
            pa = np.concatenate([np.zeros((TOK,), np.int32), pos[b, :TOK]], 0)
        else:
            xa = x[b]
            pa = pos[b]
        m['xa'] = np.ascontiguousarray(xa)
        m['posa'] = np.ascontiguousarray(pa)
        m['flag'] = np.full((128, 1), float(hf), np.float32)
        m['mem'] = np.ascontiguousarray(mem[b])
        m['gq_col'] = np.ascontiguousarray(np.tile(shared['dil_q_norm'].reshape(64), 2).reshape(128, 1))
        m['gk_col'] = np.ascontiguousarray(np.tile(shared['dil_k_norm'].reshape(64), 2).reshape(128, 1))
        maps.append(m)
    return maps


_NC = None


def kernel(**inputs):
    global _NC
    if _NC is None:
        _NC = build()[0]
    maps = make_in_maps(inputs)
    res = run_bass_kernel_spmd(_NC, maps, core_ids=list(range(8)))
    out = np.zeros((4, 4096, 1024), np.float32)
    for c in range(8):
        b, hf = c // 2, c % 2
        out[b, hf * TOK:(hf + 1) * TOK] = res.results[c]['out']
    return out
```

```python
import math
import types
from contextlib import ExitStack

import numpy as np
import concourse.bass as bass
import concourse.mybir as mybir
from concourse.bass_utils import run_bass_kernel_spmd

F32 = mybir.dt.float32
BF16 = mybir.dt.bfloat16
I32 = mybir.dt.int32
U32 = mybir.dt.uint32
AF = mybir.ActivationFunctionType
ALU = mybir.AluOpType
AX = mybir.AxisListType

EPS = 1e-6
DIL_STOP = 99
NT = 16
TOK = 2048
C_Z, C_X, C_B, C_C, C_DT, C_QD, C_KD, C_VD, C_QM, C_G = 0, 2048, 4096, 5120, 6144, 6176, 7712, 9248, 10784, 12320
K_ID, K_TRIU, K_TRIL, K_TRILS, K_ONES, K_BM, K_RM, K_SH0, K_SH1, K_LO, K_INVF, K_IOTA, K_INVFC, K_END = 0, 128, 256, 384, 512, 640, 768, 896, 1024, 1152, 1280, 1312, 1328, 1332


def _freeze(fn):
    if fn.__closure__ is None:
        return fn
    cells = []
    for c in fn.__closure__:
        try:
            cells.append(types.CellType(c.cell_contents))
        except ValueError:
            cells.append(c)
    return types.FunctionType(fn.__code__, fn.__globals__, fn.__name__, fn.__defaults__, tuple(cells))


class Buf:
    __slots__ = ('name', 'w', 'r', 'sem', 'semcnt')

    def __init__(self, name):
        self.name = name
        self.w = {}
        self.r = {}
        self.sem = None
        self.semcnt = 0


class Sched:
    ENG = ('pe', 'act', 'dve', 'pool', 'sp')

    def __init__(self, nc):
        self.nc = nc
        self.prog = {e: [] for e in self.ENG}
        self.cnt = {e: 0 for e in self.ENG}
        self.sem = {e: nc.alloc_semaphore('prog_' + e) for e in self.ENG}
        self.seen = {e: {} for e in self.ENG}
        self.dmabufs = []
        self.nwait = 0

    def _waits(self, e, toks):
        need = {}
        for key, (sem, val, src) in toks:
            if src == 'pe' and e == 'pe':
                continue
            if self.seen[e].get(key, 0) >= val:
                continue
            if key not in need or need[key][1] < val:
                need[key] = (sem, val)
        for key, (sem, val) in need.items():
            self.seen[e][key] = val
            self.prog[e].append(('wait', sem, val))
            self.nwait += 1

    @staticmethod
    def _merge(d, key, tok):
        if key not in d or d[key][1] < tok[1]:
            d[key] = tok

    @staticmethod
    def _deps(reads, writes):
        toks = []
        for b in reads:
            toks += list(b.w.items())
        for b in writes:
            toks += list(b.w.items())
            toks += list(b.r.items())
        return toks

    def op(self, e, fn, reads=(), writes=()):
        self._waits(e, self._deps(reads, writes))
        self.cnt[e] += 1
        tok = (self.sem[e], self.cnt[e], e)
        self.prog[e].append(('op', _freeze(fn), self.sem[e], 1))
        for b in reads:
            self._merge(b.r, e, tok)
        for b in writes:
            b.w = {e: tok}
            b.r = {}

    def dma(self, q, out_ap, in_ap, reads, writes, sembuf, indirect=None, **kw):
        if sembuf.sem is None:
            sembuf.sem = self.nc.alloc_semaphore('dma_' + sembuf.name)
            self.dmabufs.append(sembuf)
        key = 'dma_' + sembuf.name
        toks = self._deps(reads, writes)
        if sembuf.semcnt:
            toks.append((key, (sembuf.sem, sembuf.semcnt, None)))
        self._waits(q, toks)
        sembuf.semcnt += 16
        tok = (sembuf.sem, sembuf.semcnt, None)
        if indirect is None:
            fn = lambda e: e.dma_start(out=out_ap, in_=in_ap, **kw)
        else:
            fn = _freeze(indirect)
        self.prog[q].append(('op', fn, sembuf.sem, 16))
        for b in reads:
            self._merge(b.r, key, tok)
        for b in writes:
            b.w = {key: tok}
            b.r = {}

    def barrier(self):
        toks = []
        for e in self.ENG:
            if self.cnt[e]:
                toks.append((e, (self.sem[e], self.cnt[e], None)))
        for b in self.dmabufs:
            toks.append(('dma_' + b.name, (b.sem, b.semcnt, None)))
        for e in self.ENG:
            self._waits(e, toks)

    def release_dma_sems(self, bufs):
        pass

    def finish(self):
        self.barrier()
        nc = self.nc
        prog = self.prog

        def mk(e):
            def body(engine):
                for item in prog[e]:
                    if item[0] == 'wait':
                        engine.wait_ge(item[1], item[2])
                    else:
                        item[1](engine).then_inc(item[2], item[3])
            return body
        with nc.Block() as block:
            block.sync(mk('sp'))
            block.scalar(mk('act'))
            block.vector(mk('dve'))
            block.gpsimd(mk('pool'))
            block.tensor(mk('pe'))


def build(upto=99, dbg=()):
    nc = bass.Bass("TRN2", target_bir_lowering=False)
    S = Sched(nc)
    op = S.op

    def din(name, shape, dt=F32):
        return nc.dram_tensor(name, shape, dt, kind="ExternalInput").ap()

    xa = din('xa', [4096, 1024])
    posa = din('posa', [4096], I32)
    flag_d = din('flag', [128, 1])
    mem_d = din('mem', [256, 1024])
    cst_d = din('cst', [128, K_END])
    norm_mix = din('norm_mix', [1, 1024])
    w_in = din('w_in', [1024, 15392])
    conv_w = din('conv_w', [4, 4096])
    conv_b = din('conv_b', [1, 4096])
    dt_bias = din('dt_bias', [1, 32])
    a_log = din('a_log', [1, 32])
    d_skip = din('d_skip', [1, 32])
    ssd_norm = din('ssd_norm', [1, 2048])
    dil_q_norm = din('dil_q_norm', [1, 64])
    dil_k_norm = din('dil_k_norm', [1, 64])
    gq_col = din('gq_col', [128, 1])
    gk_col = din('gk_col', [128, 1])
    mem_norm = din('mem_norm', [1, 1024])
    w_mem_kv = din('w_mem_kv', [1024, 3072])
    mem_q_norm = din('mem_q_norm', [1, 384])
    mem_k_norm = din('mem_k_norm', [1, 384])
    w_up_ssd = din('w_up_ssd', [2048, 1024])
    w_up_dil = din('w_up_dil', [512, 1024])
    w_up_mem = din('w_up_mem', [1536, 1024])
    w_out = din('w_out', [1024, 1024])
    norm_ffn = din('norm_ffn', [1, 1024])
    peer_w_q = din('peer_w_q', [1024, 2048])
    peer_keys1 = din('peer_keys1', [128, 128])
    peer_keys2 = din('peer_keys2', [128, 128])
    peer_u = din('peer_u', [16384, 1024])
    peer_v = din('peer_v', [16384, 1024])
    out_d = nc.dram_tensor('out', [TOK, 1024], F32, kind="ExternalOutput").ap()
    ysc = nc.dram_tensor('ysc', [TOK, 2048], F32, kind="Internal").ap()
    cb16 = nc.dram_tensor('cb16', [16384, 2048], BF16, kind="Internal").ap()
    b_ysc = [[Buf('ysc%d_%d' % (t, g)) for g in range(8)] for t in range(NT)]
    dbg_out = {}
    for name, shape in dbg:
        dbg_out[name] = nc.dram_tensor('dbg_' + name, shape, F32, kind="ExternalOutput").ap()

    top = ExitStack()

    def mk_sb(stack):
        def sb(name, shape, dt=F32):
            t = stack.enter_context(nc.sbuf_tensor('s_' + name, shape, dt))
            return t, Buf(name)
        return sb
    sb0 = mk_sb(top)

    psf = [(nc.alloc_psum_tensor('psf%d' % i, [128, 512], F32), Buf('psf%d' % i)) for i in range(6)]
    psb = [(nc.alloc_psum_tensor('psb%d' % i, [128, 1024], BF16), Buf('psb%d' % i)) for i in range(2)]
    rr = {'f': 0, 'b': 0, 'cast': 0, 'stg': 0}

    rr['n'] = 6

    def ps_next():
        rr['f'] = (rr['f'] + 1) % rr['n']
        return psf[rr['f']]

    def psb_next():
        rr['b'] = (rr['b'] + 1) % 2
        return psb[rr['b']]

    cst, b_cst = sb0('cst', [128, K_END])
    S.dma('sp', cst[:], cst_d, [], [b_cst], b_cst)
    cstb, b_cstb = sb0('cstb', [128, K_INVF], BF16)
    op('dve', lambda e: e.tensor_copy(out=cstb[:], in_=cst[:, 0:K_INVF]), [b_cst], [b_cstb])
    identf = cst[:, K_ID:K_ID + 128]
    identb = cstb[:, K_ID:K_ID + 128]
    flag, b_flag = sb0('flag', [128, 1])
    S.dma('sp', flag[:], flag_d, [], [b_flag], b_flag)
    epst, b_eps = sb0('epst', [128, 2])
    op('pool', lambda e: e.memset(epst[:, 0:1], EPS), [], [b_eps])
    op('pool', lambda e: e.memset(epst[:, 1:2], 1.0), [], [b_eps])
    stg = [sb0('stg%d' % i, [128, 2048]) for i in range(2)]

    wbufs = {}
    wq_ = {'q': 'sp'}

    def load_w(dst, src, nk, ncols, P=128, name='w'):
        bufs = []
        if ncols <= 2048:
            kstep = max(1, 2048 // ncols)
            cstep = ncols
        else:
            kstep = 1
            cstep = 2048
        for k0 in range(0, nk, kstep):
            k1 = min(nk, k0 + kstep)
            for c0 in range(0, ncols, cstep):
                c1 = min(ncols, c0 + cstep)
                n = (k1 - k0) * (c1 - c0)
                rr['stg'] ^= 1
                st, b_st = stg[rr['stg']]
                sv = st[0:P, 0:n].rearrange("p (k c) -> p k c", k=k1 - k0)
                S.dma(wq_['q'], sv, src[k0 * P:k1 * P, c0:c1].rearrange("(k p) c -> p k c", p=P), [], [b_st], b_st)
                bkey = (name, k0, c0)
                if bkey not in wbufs:
                    wbufs[bkey] = Buf('%s_%d_%d' % bkey)
                b = wbufs[bkey]
                rr['cast'] = (rr['cast'] + 1) % 2
                eng = ('dve', 'act')[rr['cast']]
                dv = dst[0:P, k0:k1, c0:c1]
                if eng == 'act':
                    op(eng, lambda e, dv=dv, sv=sv: e.copy(out=dv, in_=sv), [b_st], [b])
                else:
                    op(eng, lambda e, dv=dv, sv=sv: e.tensor_copy(out=dv, in_=sv), [b_st], [b])
                bufs.append(b)
        return bufs

    def bcast_load(sbf, name, src, n):
        t, b = sbf(name, [128, n])
        S.dma('sp', t[:], src.partition_broadcast(128), [], [b], b)
        return t, b

    def rstd_from_ssq(ssq_ap, out_ap, n, rbuf):
        op('act', lambda e: e.activation(out=out_ap, in_=ssq_ap, func=AF.Sqrt, scale=1.0 / n, bias=epst[:, 0:1]), [rbuf, b_eps], [rbuf])
        op('dve', lambda e: e.reciprocal(out=out_ap, in_=out_ap), [rbuf], [rbuf])

    def dump(name, src_ap, rbuf):
        if name in dbg_out:
            S.dma('sp', dbg_out[name], src_ap, [rbuf], [], rbuf)

    msc = nc.dram_tensor('msc', [TOK, 1024], F32, kind="Internal").ap()
    b_msc = [Buf('msc%d' % t) for t in range(NT)]
    b_x1d = [Buf('x1d%d' % t) for t in range(NT)]

    mid = ExitStack()
    sb1 = mk_sb(mid)
    hTo, _ = sb1('hTo', [128, 8, TOK], BF16)
    ctxs = ExitStack()
    sbc = mk_sb(ctxs)
    hTc, _ = sbc('hTc', [128, 8, TOK], BF16)
    b_hT = [Buf('hT%d' % t) for t in range(32)]
    own = lambda t: slice(TOK + t * 128, TOK + (t + 1) * 128)

    def hsl(kc, sl):
        step = sl.step or 1
        if sl.start >= TOK:
            return hTo[:, kc, sl.start - TOK:sl.stop - TOK:step]
        assert sl.stop <= TOK
        return hTc[:, kc, sl.start:sl.stop:step]

    with ExitStack() as ph:
        sb = mk_sb(ph)
        gmix, b_gmix = bcast_load(sb, 'gmix', norm_mix, 1024)
        xts = [sb('xt%d' % i, [128, 4, 1024]) for i in range(2)]
        sq, b_sq = sb('sq', [128, 1024])
        hbr = [sb('hb%d' % i, [128, 1024], BF16) for i in range(2)]
        str_ = [sb('st%d' % i, [128, 2]) for i in range(2)]
        xa4 = xa.rearrange("(n j p) d -> n p j d", j=4, p=128)
        pend = []

        def stage1(tt):
            xt4, b_xt = xts[(tt // 4) % 2]
            if tt % 4 == 0:
                S.dma('sp', xt4[:], xa4[tt // 4], [], [b_xt], b_xt)
            xt = xt4[:, tt % 4, :]
            hb, b_hb = hbr[tt % 2]
            st, b_st = str_[tt % 2]
            op('act', lambda e: e.activation(out=sq[:], in_=xt, func=AF.Square, accum_out=st[:, 0:1]), [b_xt], [b_sq, b_st])
            rstd_from_ssq(st[:, 0:1], st[:, 1:2], 1024, b_st)
            op('dve', lambda e: e.scalar_tensor_tensor(out=hb[:], in0=xt, scalar=st[:, 1:2], in1=gmix[:], op0=ALU.mult, op1=ALU.mult),
               [b_xt, b_st, b_gmix], [b_hb])
            pb, b_pb = psb_next()
            for kc in range(8):
                op('pe', lambda e, kc=kc: e.transpose(out=pb[:, kc * 128:(kc + 1) * 128], in_=hb[:, kc * 128:(kc + 1) * 128], identity=identb),
                   [b_hb, b_cstb], [b_pb])
            pend.append((tt, pb, b_pb))

        def stage2():
            tt, pb, b_pb = pend.pop(0)
            hdst = hTc[:, :, tt * 128:(tt + 1) * 128] if tt < 16 else hTo[:, :, (tt - 16) * 128:(tt - 15) * 128]
            op('act', lambda e: e.copy(out=hdst, in_=pb[:].rearrange("p (k t) -> p k t", k=8)), [b_pb], [b_hT[tt]])
        stage1(0)
        for tt in range(1, 32):
            stage1(tt)
            stage2()
        stage2()
        S.barrier()
    if 'hT' in dbg_out:
        with ExitStack() as ph:
            sb = mk_sb(ph)
            tmp, b_tmp = sb('dbgt', [128, 4096])
            op('dve', lambda e: e.tensor_copy(out=tmp[:, 0:TOK], in_=hTc[:, 3, :]), b_hT, [b_tmp])
            op('dve', lambda e: e.tensor_copy(out=tmp[:, TOK:], in_=hTo[:, 3, :]), b_hT, [b_tmp])
            dump('hT', tmp[:], b_tmp)
            S.barrier()

    if upto >= 1:
        dts = ExitStack()
        sbd = mk_sb(dts)
        dtt, b_dtt = sbd('dtt', [128, 32, 32])
        at, b_at = sbd('at', [128, 32, 32])
        ecs, b_ecs = sbd('ecs', [128, 32, 32])
        etot, b_etot = sbd('etot', [128, 32, 32])
        wst, b_wst = sbd('wst', [128, 32, 32])
        dskb, b_dskb = bcast_load(sbd, 'dskb', d_skip, 32)
        with ExitStack() as ph:
            sb = mk_sb(ph)
            wdt, _ = sb('wdt', [128, 8, 32], BF16)
            bw = load_w(wdt, w_in[:, C_DT:C_DT + 32], 8, 32, name='wdt')
            dtbb, b_dtbb = bcast_load(sb, 'dtbb', dt_bias, 32)
            alb, b_alb = bcast_load(sb, 'alb', a_log, 32)
            t0, b_t0 = sb('dt_t0', [128, 32, 32])
            t1, b_t1 = sb('dt_t1', [128, 32, 32])
            op('act', lambda e: e.activation(out=alb[:], in_=alb[:], func=AF.Exp), [b_alb], [b_alb])
            for half in range(2):
                p, b_p = ps_next()
                for j in range(16):
                    tt = half * 16 + j
                    for kc in range(8):
                        op('pe', lambda e, p=p, j=j, tt=tt, kc=kc: e.matmul(p[:, j * 32:(j + 1) * 32], lhsT=hsl(kc, slice(tt * 128, (tt + 1) * 128)),
                                                                           rhs=wdt[:, kc, :], start=(kc == 0), stop=(kc == 7)),
                           [b_hT[tt]] + bw, [b_p])
                op('dve', lambda e, p=p, half=half: e.tensor_tensor(out=t0[:, half * 16:(half + 1) * 16, :], in0=p[:].rearrange("p (j h) -> p j h", j=16),
                                                                    in1=dtbb[:].unsqueeze(1).to_broadcast([128, 16, 32]), op=ALU.add),
                   [b_p, b_dtbb], [b_t0])
            op('act', lambda e: e.activation(out=t1[:], in_=t0[:], func=AF.Abs), [b_t0], [b_t1])
            op('act', lambda e: e.activation(out=t1[:], in_=t1[:], func=AF.Exp, scale=-1.0), [b_t1], [b_t1])
            op('act', lambda e: e.activation(out=t1[:], in_=t1[:], func=AF.Ln, bias=epst[:, 1:2]), [b_t1, b_eps], [b_t1])
            op('dve', lambda e: e.scalar_tensor_tensor(out=dtt[:], in0=t0[:], scalar=0.0, in1=t1[:], op0=ALU.max, op1=ALU.add), [b_t0, b_t1], [b_dtt])
            op('dve', lambda e: e.scalar_tensor_tensor(out=at[:], in0=dtt[:], scalar=-1.0, in1=alb[:].unsqueeze(1).to_broadcast([128, 32, 32]),
                                                       op0=ALU.mult, op1=ALU.mult), [b_dtt, b_alb], [b_at])
            atf = at[:].rearrange("p c h -> p (c h)")
            for half in range(2):
                sl = slice(half * 512, (half + 1) * 512)
                pc, b_pc = ps_next()
                op('pe', lambda e, pc=pc, sl=sl: e.matmul(pc[:], lhsT=cst[:, K_TRIU:K_TRIU + 128], rhs=atf[:, sl], start=True, stop=True), [b_at, b_cst], [b_pc])
                pt, b_pt = ps_next()
                op('pe', lambda e, pt=pt, sl=sl: e.matmul(pt[:], lhsT=cst[:, K_ONES:K_ONES + 128], rhs=atf[:, sl], start=True, stop=True), [b_at, b_cst], [b_pt])
                t0f = t0[:].rearrange("p c h -> p (c h)")
                op('act', lambda e, pc=pc, sl=sl: e.activation(out=ecs[:].rearrange("p c h -> p (c h)")[:, sl], in_=pc[:], func=AF.Exp), [b_pc], [b_ecs])
                op('act', lambda e, pt=pt, sl=sl: e.activation(out=etot[:].rearrange("p c h -> p (c h)")[:, sl], in_=pt[:], func=AF.Exp), [b_pt], [b_etot])
                op('act', lambda e, pc=pc, sl=sl, t0f=t0f: e.copy(out=t0f[:, sl], in_=pc[:]), [b_pc, b_t0], [b_t0])
                op('dve', lambda e, pt=pt, sl=sl, t0f=t0f: e.tensor_tensor(out=t0f[:, sl], in0=pt[:], in1=t0f[:, sl], op=ALU.subtract), [b_pt, b_t0], [b_t0])
            op('act', lambda e: e.activation(out=t0[:], in_=t0[:], func=AF.Exp), [b_t0], [b_t0])
            op('dve', lambda e: e.tensor_tensor(out=wst[:], in0=t0[:], in1=dtt[:], op=ALU.mult), [b_t0, b_dtt], [b_wst])
            dump('dtt', dtt[:].rearrange("p c h -> p (c h)"), b_dtt)
            dump('ecs', ecs[:].rearrange("p c h -> p (c h)"), b_ecs)
            dump('wst', wst[:].rearrange("p c h -> p (c h)"), b_wst)
            S.barrier()

    if upto >= 2:
        with ExitStack() as ph:
            sb = mk_sb(ph)
            cwr, b_cwr = sb('cwr', [128, 128])
            S.dma('sp', cwr[:], conv_w.rearrange("k (n p) -> (k n) p", p=128), [], [b_cwr], b_cwr)
            cbr, b_cbr = sb('cbr', [32, 128])
            S.dma('sp', cbr[:], conv_b.rearrange("o (n p) -> (o n) p", p=128), [], [b_cbr], b_cbr)
            cwT, b_cwT = sb('cwT', [128, 4, 32])
            cbT, b_cbT = sb('cbT', [128, 32])
            p, b_p = ps_next()
            op('pe', lambda e, p=p: e.transpose(out=p[:, 0:128], in_=cwr[:], identity=identf), [b_cwr, b_cst], [b_p])
            op('pe', lambda e, p=p: e.transpose(out=p[:, 128:160], in_=cbr[:], identity=cst[0:32, K_ID:K_ID + 32]), [b_cbr, b_cst], [b_p])
            op('act', lambda e, p=p: e.copy(out=cwT[:].rearrange("p k n -> p (k n)"), in_=p[:, 0:128]), [b_p], [b_cwT])
            op('act', lambda e, p=p: e.copy(out=cbT[:], in_=p[:, 128:160]), [b_p], [b_cbT])
            wg, _ = sb('wg', [128, 8, 512], BF16)
            raws = [sb('raw%d' % j, [128, 515]) for j in range(4)]
            cacc, b_cacc = sb('cacc', [128, 512])
            xbcT, _ = sb('xbcT', [128, 4, 4096], BF16)
            b_xbc = [[Buf('xbc%d_%d' % (j, s)) for s in range(8)] for j in range(4)]
            xtokr = [sb('xtok%d' % i, [128, 384], BF16) for i in range(3)]
            xsr = [sb('xs%d' % i, [128, 256], BF16) for i in range(3)]
            xdr = [sb('xd%d' % i, [128, 256], BF16) for i in range(3)]
            cbmr = [sb('cbm%d' % i, [128, 128]) for i in range(3)]
            arhsr = [sb('arhs%d' % i, [128, 4, 128]) for i in range(2)]
            decr = [sb('dec%d' % i, [128, 4, 128]) for i in range(2)]
            MTr = [sb('MT%d' % i, [128, 4, 128], BF16) for i in range(3)]
            t2sr = [sb('t2s%d' % i, [128, 256]) for i in range(3)]
            t1sr = [sb('t1s%d' % i, [128, 256]) for i in range(2)]
            ybufs = [sb('ybuf%d' % i, [128, 256]) for i in range(2)]
            Sp, b_Sp = sb('Sprev', [128, 256])
            Stmp, b_Stmp = sb('Stmp', [128, 256])
            Sbfr = [sb('Sbf%d' % i, [128, 256], BF16) for i in range(2)]
            triu_f = cst[:, K_TRIU:K_TRIU + 128]
            NCV = 3
            cvi = [sb('cvi%d' % i, [128, 1024]) for i in range(NCV)]
            cvo = [sb('cvo%d' % i, [128, 1024], BF16) for i in range(NCV)]
            b_cb16 = Buf('cb16')
            tabs = (peer_u.rearrange("(p r) d -> p r d", r=128), peer_v.rearrange("(p r) d -> p r d", r=128))
            c_v = cb16.rearrange("(p r) d -> p r d", r=128)

            def convert_step(q):
                if q < 256:
                    ci_, b_ci = cvi[q % NCV]
                    S.dma('sp', ci_[:], tabs[q % 2][:, q // 2, :], [], [b_ci], b_ci)
                if 0 <= q - 1 < 256:
                    ci_, b_ci = cvi[(q - 1) % NCV]
                    co_, b_co = cvo[(q - 1) % NCV]
                    op('act', lambda e: e.copy(out=co_[:], in_=ci_[:]), [b_ci], [b_co])
                if 0 <= q - 2 < 256:
                    co_, b_co = cvo[(q - 2) % NCV]
                    w_ = (q - 2) % 2
                    S.dma('sp', c_v[:, (q - 2) // 2, w_ * 1024:(w_ + 1) * 1024], co_[:], [b_co], [b_cb16], b_co)
            conv_r = [0]
            for g in range(8):
                bw = load_w(wg[:, :, 0:256], w_in[:, C_X + g * 256:C_X + (g + 1) * 256], 8, 256, name='wgx')
                bw += load_w(wg[:, :, 256:384], w_in[:, C_B + g * 128:C_B + (g + 1) * 128], 8, 128, name='wgb')
                bw += load_w(wg[:, :, 384:512], w_in[:, C_C + g * 128:C_C + (g + 1) * 128], 8, 128, name='wgc')
                nidx = [2 * g, 2 * g + 1, 16 + g, 24 + g]
                for j in range(4):
                    raw, b_raw = raws[j]
                    op('pool', lambda e, raw=raw: e.memset(raw[:, 0:3], 0.0), [], [b_raw])
                op('pool', lambda e: e.memset(Sp[:], 0.0), [], [b_Sp])
                op('pool', lambda e: e.memset(Sbfr[0][0][:], 0.0), [], [Sbfr[0][1]])
                for s in range(8):
                    for j in range(4):
                        if j == 3 and s < 3:
                            continue
                        raw, b_raw = raws[j]
                        n = nidx[j]
                        p, b_p = ps_next()
                        for kc in range(8):
                            op('pe', lambda e, p=p, kc=kc, j=j, s=s: e.matmul(p[:], lhsT=wg[:, kc, j * 128:(j + 1) * 128], rhs=hsl(kc, slice(s * 512, (s + 1) * 512)),
                                                                           start=(kc == 0), stop=(kc == 7)),
                               b_hT[4 * s:4 * s + 4] + bw, [b_p])
                        op('act', lambda e, p=p, raw=raw: e.copy(out=raw[:, 3:515], in_=p[:]), [b_p], [b_raw])
                        op('dve', lambda e, raw=raw, n=n: e.tensor_scalar(out=cacc[:], in0=raw[:, 3:515], scalar1=cwT[:, 3, n:n + 1], scalar2=cbT[:, n:n + 1],
                                                                         op0=ALU.mult, op1=ALU.add), [b_raw, b_cwT, b_cbT], [b_cacc])
                        for k in range(3):
                            op('dve', lambda e, raw=raw, n=n, k=k: e.scalar_tensor_tensor(out=cacc[:], in0=raw[:, k:k + 512], scalar=cwT[:, k, n:n + 1], in1=cacc[:],
                                                                                          op0=ALU.mult, op1=ALU.add), [b_raw, b_cwT, b_cacc], [b_cacc])
                        op('act', lambda e, j=j, s=s: e.activation(out=xbcT[:, j, s * 512:(s + 1) * 512], in_=cacc[:], func=AF.Silu), [b_cacc], [b_xbc[j][s]])
                        op('pool', lambda e, raw=raw: e.tensor_copy(out=raw[:, 0:3], in_=raw[:, 512:515]), [b_raw], [b_raw])
                if g == 0:
                    for j in range(4):
                        if ('xbc%d' % j) in dbg_out:
                            tmpd, b_tmpd = sb('dbgx%d' % j, [128, 4096])
                            op('dve', lambda e, j=j, tmpd=tmpd: e.tensor_copy(out=tmpd[:], in_=xbcT[:, j, :]), b_xbc[j], [b_tmpd])
                            dump('xbc%d' % j, tmpd[:], b_tmpd)
                rr['n'] = 4
                hs = slice(4 * g, 4 * g + 4)
                pSr = {}

                def frontA(c):
                    s_ = c // 4
                    tk = slice(c * 128, (c + 1) * 128)
                    xtok, b_xtok = xtokr[c % 3]
                    pb, b_pb = psb_next()
                    if c >= 16:
                        arhs, b_arhs = arhsr[c % 2]
                        op('pool', lambda e: e.tensor_tensor(out=arhs[:], in0=triu_f.unsqueeze(1).to_broadcast([128, 4, 128]),
                                                             in1=at[:, c, hs].unsqueeze(2).to_broadcast([128, 4, 128]), op=ALU.mult),
                           [b_cst, b_at], [b_arhs])
                    for j in range(3):
                        op('pe', lambda e, j=j: e.transpose(out=pb[:, j * 128:(j + 1) * 128], in_=xbcT[:, j, tk], identity=identb),
                           [b_xbc[j][s_], b_cstb], [b_pb])
                    op('act', lambda e: e.copy(out=xtok[:], in_=pb[:, 0:384]), [b_pb], [b_xtok])
                    if c >= 16:
                        dec, b_dec = decr[c % 2]
                        pG, b_pG = ps_next()
                        op('pe', lambda e: e.matmul(pG[:], lhsT=cst[:, K_TRILS:K_TRILS + 128], rhs=arhs[:].rearrange("p h l -> p (h l)"), start=True, stop=True),
                           [b_arhs, b_cst], [b_pG])
                        op('act', lambda e: e.activation(out=dec[:].rearrange("p h l -> p (h l)"), in_=pG[:], func=AF.Exp), [b_pG], [b_dec])
                    if conv_r[0] < 258:
                        convert_step(conv_r[0])
                        conv_r[0] += 1

                def frontB(c):
                    s_ = c // 4
                    tk = slice(c * 128, (c + 1) * 128)
                    xtok, b_xtok = xtokr[c % 3]
                    xs, b_xs = xsr[c % 3]
                    xv = xtok[:, 0:256].rearrange("p (h d) -> p h d", h=4)
                    op('dve', lambda e: e.tensor_tensor(out=xs[:].rearrange("p (h d) -> p h d", h=4), in0=xv,
                                                         in1=wst[:, c, hs].unsqueeze(2).to_broadcast([128, 4, 64]), op=ALU.mult),
                       [b_xtok, b_wst], [b_xs])
                    pS, b_pS = psf[4 + c % 2]
                    pSr[c] = (pS, b_pS)
                    op('pe', lambda e: e.matmul(pS[:, 0:256], lhsT=xtok[:, 256:384], rhs=xs[:], start=True, stop=True), [b_xtok, b_xs], [b_pS])
                    if c >= 16:
                        xd, b_xd = xdr[c % 3]
                        cbm, b_cbm = cbmr[c % 3]
                        dec, b_dec = decr[c % 2]
                        MT, b_MT = MTr[c % 3]
                        t2s, b_t2s = t2sr[c % 3]
                        op('dve', lambda e: e.tensor_tensor(out=xd[:].rearrange("p (h d) -> p h d", h=4), in0=xv,
                                                             in1=dtt[:, c, hs].unsqueeze(2).to_broadcast([128, 4, 64]), op=ALU.mult),
                           [b_xtok, b_dtt], [b_xd])
                        pC, b_pC = ps_next()
                        op('pe', lambda e: e.matmul(pC[:, 0:128], lhsT=xbcT[:, 2, tk], rhs=xbcT[:, 3, tk], start=True, stop=True),
                           [b_xbc[2][s_], b_xbc[3][s_]], [b_pC])
                        op('dve', lambda e: e.tensor_tensor(out=cbm[:], in0=pC[:, 0:128], in1=triu_f, op=ALU.mult), [b_pC, b_cst], [b_cbm])
                        op('dve', lambda e: e.tensor_tensor(out=MT[:], in0=dec[:], in1=cbm[:].unsqueeze(1).to_broadcast([128, 4, 128]), op=ALU.mult),
                           [b_dec, b_cbm], [b_MT])
                        op('pool', lambda e: e.tensor_tensor(out=t2s[:].rearrange("p (h d) -> p h d", h=4), in0=xv,
                                                             in1=dskb[:, hs].unsqueeze(2).to_broadcast([128, 4, 64]), op=ALU.mult),
                           [b_xtok, b_dskb], [b_t2s])

                def tail(c):
                    s_ = c // 4
                    tk = slice(c * 128, (c + 1) * 128)
                    pS, b_pS = pSr.pop(c)
                    Sbf, b_Sbf = Sbfr[c % 2]
                    Sbn, b_Sbn = Sbfr[(c + 1) % 2]
                    op('dve', lambda e: e.tensor_tensor(out=Stmp[:].rearrange("p (h d) -> p h d", h=4), in0=Sp[:].rearrange("p (h d) -> p h d", h=4),
                                                        in1=etot[:, c, hs].unsqueeze(2).to_broadcast([128, 4, 64]), op=ALU.mult),
                       [b_Sp, b_etot], [b_Stmp])
                    if c == 15:
                        op('dve', lambda e: e.tensor_tensor(out=Stmp[:], in0=pS[:, 0:256], in1=Stmp[:], op=ALU.add), [b_pS, b_Stmp], [b_Stmp])
                        op('dve', lambda e: e.tensor_scalar(out=Sp[:], in0=Stmp[:], scalar1=flag[:, 0:1], scalar2=None, op0=ALU.mult), [b_Stmp, b_flag], [b_Sp])
                    else:
                        op('dve', lambda e: e.tensor_tensor(out=Sp[:], in0=pS[:, 0:256], in1=Stmp[:], op=ALU.add), [b_pS, b_Stmp], [b_Sp])
                    op('act', lambda e: e.copy(out=Sbn[:], in_=Sp[:]), [b_Sp], [b_Sbn])
                    if c >= 16:
                        t = c - 16
                        xd, b_xd = xdr[c % 3]
                        MT, b_MT = MTr[c % 3]
                        t2s, b_t2s = t2sr[c % 3]
                        t1s, b_t1s = t1sr[c % 2]
                        pY, b_pY = ps_next()
                        for h in range(4):
                            op('pe', lambda e, h=h: e.matmul(pY[:, h * 64:(h + 1) * 64], lhsT=MT[:, h, :], rhs=xd[:, h * 64:(h + 1) * 64], start=True, stop=True),
                               [b_MT, b_xd], [b_pY])
                        pO, b_pO = ps_next()
                        op('pe', lambda e: e.matmul(pO[:, 0:256], lhsT=xbcT[:, 3, tk], rhs=Sbf[:], start=True, stop=True), [b_xbc[3][s_], b_Sbf], [b_pO])
                        op('dve', lambda e: e.tensor_tensor(out=t1s[:].rearrange("p (h d) -> p h d", h=4), in0=pO[:, 0:256].rearrange("p (h d) -> p h d", h=4),
                                                            in1=ecs[:, c, hs].unsqueeze(2).to_broadcast([128, 4, 64]), op=ALU.mult),
                           [b_pO, b_ecs], [b_t1s])
                        yb, b_yb = ybufs[t % 2]
                        op('dve', lambda e: e.tensor_tensor(out=t1s[:], in0=pY[:, 0:256], in1=t1s[:], op=ALU.add), [b_pY, b_t1s], [b_t1s])
                        op('pool', lambda e: e.tensor_tensor(out=yb[:], in0=t1s[:], in1=t2s[:], op=ALU.add), [b_t1s, b_t2s], [b_yb])
                        S.dma('sp', ysc[t * 128:(t + 1) * 128, g * 256:(g + 1) * 256], yb[:], [b_yb], [b_ysc[t][g]], b_yb)

                frontA(0)
                frontA(1)
                frontB(0)
                for c in range(32):
                    tail(c)
                    if c + 1 < 32:
                        frontB(c + 1)
                    if c + 2 < 32:
                        frontA(c + 2)
                rr['n'] = 6
            while conv_r[0] < 258:
                convert_step(conv_r[0])
                conv_r[0] += 1
            S.barrier()

    def gate_and_merge(t, pU, b_pU, wg_t, bwg, first, sg, b_sg, tmpm, b_tmpm, mtile, b_mtile):
        if not first:
            S.dma('sp', mtile[:], msc[t * 128:(t + 1) * 128, :], [b_msc[t]], [b_mtile], b_mtile)
        for nb in range(2):
            pg, b_pg = ps_next()
            for kc in range(8):
                op('pe', lambda e, pg=pg, kc=kc, nb=nb: e.matmul(pg[:], lhsT=hsl(kc, own(t)), rhs=wg_t[:, kc, nb * 512:(nb + 1) * 512], start=(kc == 0), stop=(kc == 7)),
                   [b_hT[16 + t]] + bwg, [b_pg])
            op('act', lambda e, pg=pg, nb=nb: e.activation(out=sg[:, nb * 512:(nb + 1) * 512], in_=pg[:], func=AF.Sigmoid), [b_pg], [b_sg])
            op('dve', lambda e, nb=nb: e.tensor_tensor(out=tmpm[:, nb * 512:(nb + 1) * 512], in0=pU[nb][:], in1=sg[:, nb * 512:(nb + 1) * 512], op=ALU.mult),
               [b_pU[nb], b_sg], [b_tmpm])
        if not first:
            op('pool', lambda e: e.tensor_tensor(out=tmpm[:], in0=tmpm[:], in1=mtile[:], op=ALU.add), [b_tmpm, b_mtile], [b_tmpm])
        S.dma('sp', msc[t * 128:(t + 1) * 128, :], tmpm[:], [b_tmpm], [b_msc[t]], b_tmpm)

    def phase_ssd_final():
        with ExitStack() as ph:
            sb = mk_sb(ph)
            wz, _ = sb('wz', [128, 8, 2048], BF16)
            bwz = load_w(wz, w_in[:, C_Z:C_Z + 2048], 8, 2048, name='wz')
            wg0, _ = sb('wg0', [128, 8, 1024], BF16)
            bwg0 = load_w(wg0, w_in[:, C_G:C_G + 1024], 8, 1024, name='wg0')
            wus, _ = sb('wus', [128, 16, 1024], BF16)
            bwus = load_w(wus, w_up_ssd, 16, 1024, name='wus')
            gssd, b_gssd = bcast_load(sb, 'gssd', ssd_norm, 2048)
            ytr = [sb('yt%d' % i, [128, 2048]) for i in range(2)]
            szr = [sb('sz%d' % i, [128, 2048]) for i in range(2)]
            ynr = [sb('yn%d' % i, [128, 2048], BF16) for i in range(2)]
            yTr = [sb('yT%d' % i, [128, 16, 128], BF16) for i in range(2)]
            str3 = [sb('st3_%d' % i, [128, 2]) for i in range(2)]
            sg, b_sg = sb('sg', [128, 1024])
            tmpm, b_tmpm = sb('tmpm3', [128, 1024])
            mtile, b_mtile = sb('mtile3', [128, 1024])
            live = {}

            def stA(t):
                yt, b_yt = ytr[t % 2]
                sz, b_sz = szr[t % 2]
                S.dma('sp', yt[:], ysc[t * 128:(t + 1) * 128, :], b_ysc[t], [b_yt], b_yt)
                for nb in range(4):
                    p, b_p = ps_next()
                    for kc in range(8):
                        op('pe', lambda e, kc=kc, nb=nb: e.matmul(p[:], lhsT=hsl(kc, own(t)), rhs=wz[:, kc, nb * 512:(nb + 1) * 512], start=(kc == 0), stop=(kc == 7)),
                           [b_hT[16 + t]] + bwz, [b_p])
                    op('act', lambda e, nb=nb: e.activation(out=sz[:, nb * 512:(nb + 1) * 512], in_=p[:], func=AF.Silu), [b_p], [b_sz])

            def stB(t):
                yt, b_yt = ytr[t % 2]
                sz, b_sz = szr[t % 2]
                yn, b_yn = ynr[t % 2]
                st, b_st = str3[t % 2]
                if t == 0:
                    dump('yraw', yt[:], b_yt)
                op('dve', lambda e: e.tensor_tensor(out=yt[:], in0=yt[:], in1=sz[:], op=ALU.mult), [b_yt, b_sz], [b_yt])
                op('act', lambda e: e.activation(out=sz[:], in_=yt[:], func=AF.Square, accum_out=st[:, 0:1]), [b_yt], [b_sz, b_st])
                rstd_from_ssq(st[:, 0:1], st[:, 1:2], 2048, b_st)
                op('dve', lambda e: e.scalar_tensor_tensor(out=yn[:], in0=yt[:], scalar=st[:, 1:2], in1=gssd[:], op0=ALU.mult, op1=ALU.mult),
                   [b_yt, b_st, b_gssd], [b_yn])
                pbs = []
                for half in range(2):
                    pb, b_pb = psb_next()
                    for k in range(8):
                        kc = half * 8 + k
                        op('pe', lambda e, k=k, kc=kc: e.transpose(out=pb[:, k * 128:(k + 1) * 128], in_=yn[:, kc * 128:(kc + 1) * 128], identity=identb),
                           [b_yn, b_cstb], [b_pb])
                    pbs.append((pb, b_pb))
                live[t] = pbs

            def stC(t):
                yT, b_yT = yTr[t % 2]
                for half, (pb, b_pb) in enumerate(live.pop(t)):
                    op('act', lambda e, half=half: e.copy(out=yT[:, half * 8:(half + 1) * 8, :], in_=pb[:].rearrange("p (k t) -> p k t", k=8)), [b_pb], [b_yT])
                pU = []
                b_pU = []
                for nb in range(2):
                    p, b_p = ps_next()
                    for kc in range(16):
                        op('pe', lambda e, kc=kc, nb=nb: e.matmul(p[:], lhsT=yT[:, kc, :], rhs=wus[:, kc, nb * 512:(nb + 1) * 512], start=(kc == 0), stop=(kc == 15)),
                           [b_yT] + bwus, [b_p])
                    pU.append(p)
                    b_pU.append(b_p)
                gate_and_merge(t, pU, b_pU, wg0, bwg0, False, sg, b_sg, tmpm, b_tmpm, mtile, b_mtile)
            stages = (stA, stB, stC)
            for step in range(NT + 2):
                for j in (2, 1, 0):
                    t = step - j
                    if 0 <= t < NT:
                        stages[j](t)
            S.barrier()

    def phase_dil():
        with ExitStack() as ph:
            sb = mk_sb(ph)
            ydT, b_ydT = sb('ydT', [128, 4, TOK], BF16)
            gqc, b_gqc = sb('gqc', [128, 1])
            gkc, b_gkc = sb('gkc', [128, 1])
            S.dma('sp', gqc[:], gq_col, [], [b_gqc], b_gqc)
            S.dma('sp', gkc[:], gk_col, [], [b_gkc], b_gkc)
            NH = 2
            with ExitStack() as ph2:
                sb2 = mk_sb(ph2)
                cosF, b_cosF = sb2('cosF', [128, 4096], BF16)
                sinF, b_sinF = sb2('sinF', [128, 4096], BF16)
                with ExitStack() as ph3:
                    sb3 = mk_sb(ph3)
                    posb, b_posb = sb3('posb', [128, 2048], I32)
                    angF, b_angF = sb3('angF', [128, 2048])
                    tmpF, b_tmpF = sb3('tmpF', [128, 2048])
                    kiF, b_kiF = sb3('kiF', [128, 2048], I32)
                    kfF, b_kfF = sb3('kfF', [128, 2048])
                    TWO_PI = 2 * math.pi
                    PCL = 3.141592
                    posa2 = posa.rearrange("(o n) -> o n", o=1)
                    for hf_ in range(2):
                        tsl_ = slice(hf_ * 2048, (hf_ + 1) * 2048)
                        S.dma('sp', posb[:], posa2[:, tsl_].partition_broadcast(128), [], [b_posb], b_posb)
                        op('dve', lambda e: e.tensor_copy(out=angF[:], in_=posb[:]), [b_posb], [b_angF])
                        op('dve', lambda e: e.tensor_scalar(out=angF[:], in0=angF[:], scalar1=cst[:, K_INVFC:K_INVFC + 1], scalar2=None, op0=ALU.mult), [b_angF, b_cst], [b_angF])
                        for dst, b_dst, shift in ((sinF, b_sinF, 0.0), (cosF, b_cosF, 0.5 * math.pi)):
                            op('dve', lambda e: e.tensor_scalar(out=tmpF[:], in0=angF[:], scalar1=shift, scalar2=None, op0=ALU.add), [b_angF], [b_tmpF])
                            op('dve', lambda e: e.tensor_scalar(out=kiF[:], in0=tmpF[:], scalar1=1.0 / TWO_PI, scalar2=None, op0=ALU.mult), [b_tmpF], [b_kiF])
                            op('dve', lambda e: e.tensor_copy(out=kfF[:], in_=kiF[:]), [b_kiF], [b_kfF])
                            op('dve', lambda e: e.scalar_tensor_tensor(out=tmpF[:], in0=kfF[:], scalar=-TWO_PI, in1=tmpF[:], op0=ALU.mult, op1=ALU.add), [b_kfF, b_tmpF], [b_tmpF])
                            op('dve', lambda e: e.tensor_scalar(out=tmpF[:], in0=tmpF[:], scalar1=-PCL, scalar2=PCL, op0=ALU.max, op1=ALU.min), [b_tmpF], [b_tmpF])
                            op('act', lambda e: e.activation(out=dst[:, tsl_], in_=tmpF[:], func=AF.Sin), [b_tmpF], [b_dst])
                    S.barrier()
                if 'cosF' in dbg_out:
                    tmpd, b_tmpd = sb2('dbgc', [128, 4096])
                    op('dve', lambda e: e.tensor_copy(out=tmpd[:], in_=cosF[:]), [b_cosF], [b_tmpd])
                    dump('cosF', tmpd[:], b_tmpd)
                if DIL_STOP <= 1:
                    S.barrier()
                    return
                acc, b_acc = sb2('acc', [65, NH, TOK])
                wd, _ = sb2('wd', [128, 8, 384], BF16)
                kTf, _ = sb2('kTf', [128, 4096], BF16)
                qTf, _ = sb2('qTf', [128, TOK], BF16)
                b_kTf = [Buf('kTf%d' % i) for i in range(8)]
                b_qTf = [Buf('qTf%d' % i) for i in range(4)]
                Vt, _ = sb2('Vt', [128, 32, NH, 65], BF16)
                vTf, _ = sb2('vTf', [128, 4096], BF16)
                b_vTf = [Buf('vTf%d' % i) for i in range(8)]
                b_Vt = [Buf('Vt%d' % i) for i in range(32)]
                b_Vones = Buf('Vones')
                sqbr = [sb2('sqb%d' % i, [128, 512], BF16) for i in range(2)]
                rsr = [sb2('rs%d' % i, [128, 512]) for i in range(2)]
                qnr = [sb2('qn%d' % i, [128, 512], BF16) for i in range(2)]
                rar = [sb2('ra%d' % i, [128, 512]) for i in range(2)]
                rbr = [sb2('rb%d' % i, [128, 512]) for i in range(2)]
                pTr = [sb2('pT%d' % i, [128, 4, 128], BF16) for i in range(3)]
                maskc, b_maskc = sb2('maskc', [128, 4, 128], BF16)
                rec, b_rec = sb2('rec', [128, 512])
                op('pool', lambda e: e.memset(Vt[:, :, :, 64:65], 1.0), [], [b_Vones])
                for i_ in range(4):
                    src_ = cstb[:, K_TRIL:K_TRIL + 128] if i_ % 2 == 0 else cstb[:, K_TRIU:K_TRIU + 128]
                    op('pool', lambda e: e.tensor_copy(out=maskc[:, i_, :], in_=src_), [b_cstb, b_maskc], [b_maskc])
                Bm = cstb[:, K_BM:K_BM + 128]
                Rm = cstb[:, K_RM:K_RM + 128]
                uctr = [0]

                def qk_stages(wcol, gcol, b_gcol, start, n, dstT, dst_off, b_dst, bw):
                    u = uctr[0] % 2
                    uctr[0] += 1
                    sqb, b_sqb = sqbr[u]
                    rs, b_rs = rsr[u]
                    qn, b_qn = qnr[u]
                    ra, b_ra = rar[u]
                    rb, b_rb = rbr[u]
                    hb_ = [b_hT[x] for x in range(start // 128, (start + n - 1) // 128 + 1)]
                    st = {}

                    def stA():
                        p1, b_p1 = ps_next()
                        st['p1'] = (p1, b_p1)
                        for kc in range(8):
                            op('pe', lambda e, kc=kc: e.matmul(p1[:, 0:n], lhsT=wd[:, kc, wcol:wcol + 128], rhs=hsl(kc, slice(start, start + n)), start=(kc == 0), stop=(kc == 7)),
                               hb_ + bw, [b_p1])
                        op('act', lambda e: e.activation(out=sqb[:, 0:n], in_=p1[:, 0:n], func=AF.Square), [b_p1], [b_sqb])

                    def stB():
                        p1, b_p1 = st['p1']
                        p2, b_p2 = ps_next()
                        op('pe', lambda e: e.matmul(p2[:, 0:n], lhsT=Bm, rhs=sqb[:, 0:n], start=True, stop=True), [b_sqb, b_cstb], [b_p2])
                        op('act', lambda e: e.activation(out=rs[:, 0:n], in_=p2[:, 0:n], func=AF.Sqrt, scale=1.0 / 64, bias=epst[:, 0:1]), [b_p2, b_eps], [b_rs])
                        op('dve', lambda e: e.reciprocal(out=rs[:, 0:n], in_=rs[:, 0:n]), [b_rs], [b_rs])
                        op('dve', lambda e: e.scalar_tensor_tensor(out=qn[:, 0:n], in0=p1[:, 0:n], scalar=gcol[:, 0:1], in1=rs[:, 0:n], op0=ALU.mult, op1=ALU.mult),
                           [b_p1, b_gcol, b_rs], [b_qn])

                    def stC():
                        p3, b_p3 = ps_next()
                        op('pe', lambda e: e.matmul(p3[:, 0:n], lhsT=Rm, rhs=qn[:, 0:n], start=True, stop=True), [b_qn, b_cstb], [b_p3])
                        op('pool', lambda e: e.tensor_tensor(out=ra[:, 0:n], in0=qn[:, 0:n], in1=cosF[:, start:start + n], op=ALU.mult), [b_qn, b_cosF], [b_ra])
                        op('dve', lambda e: e.tensor_tensor(out=rb[:, 0:n], in0=p3[:, 0:n], in1=sinF[:, start:start + n], op=ALU.mult), [b_p3, b_sinF], [b_rb])
                        op('dve', lambda e: e.tensor_tensor(out=dstT[:, start - dst_off:start - dst_off + n], in0=ra[:, 0:n], in1=rb[:, 0:n], op=ALU.add), [b_ra, b_rb], [b_dst])
                    return [stA, stB, stC]

                def v_stage(start, n, bw):
                    hb_ = [b_hT[x] for x in range(start // 128, (start + n - 1) // 128 + 1)]

                    def stA():
                        p1, b_p1 = ps_next()
                        for kc in range(8):
                            op('pe', lambda e, kc=kc: e.matmul(p1[:, 0:n], lhsT=wd[:, kc, 256:384], rhs=hsl(kc, slice(start, start + n)), start=(kc == 0), stop=(kc == 7)),
                               hb_ + bw, [b_p1])
                        op('act', lambda e: e.copy(out=vTf[:, start:start + n], in_=p1[:, 0:n]), [b_p1], [b_vTf[start // 512]])
                    return [stA]

                def run_skewed(units):
                    nst = max(len(u_) for u_ in units)
                    for step in range(len(units) + nst - 1):
                        for j in range(nst):
                            i = step - j
                            if 0 <= i < len(units) and j < len(units[i]):
                                units[i][j]()

                for hp in range(4):
                    for g, d in enumerate((1, 4, 16)):
                        nb = 32 // d
                        nh = nb // 2
                        co = g * 512 + hp * 128
                        bw = load_w(wd[:, :, 0:128], w_in[:, C_QD + co:C_QD + co + 128], 8, 128, name='wdq')
                        bw += load_w(wd[:, :, 128:256], w_in[:, C_KD + co:C_KD + co + 128], 8, 128, name='wdk')
                        bw += load_w(wd[:, :, 256:384], w_in[:, C_VD + co:C_VD + co + 128], 8, 128, name='wdv')
                        cstart = TOK - 128 * d
                        kunits = []
                        s0 = cstart
                        while s0 < TOK:
                            n_ = min(512, TOK - s0)
                            kunits.append((s0, n_))
                            s0 += n_
                        units = []
                        for (s0, n_) in kunits:
                            units.append(qk_stages(128, gkc, b_gkc, s0, n_, kTf, 0, b_kTf[s0 // 512], bw))
                            units.append(v_stage(s0, n_, bw))
                        for sl in range(4):
                            units.append(v_stage(TOK + sl * 512, 512, bw))
                            units.append(qk_stages(128, gkc, b_gkc, TOK + sl * 512, 512, kTf, 0, b_kTf[4 + sl], bw))
                            units.append(qk_stages(0, gqc, b_gqc, TOK + sl * 512, 512, qTf, TOK, b_qTf[sl], bw))
                        run_skewed(units)
                        if DIL_STOP <= 2:
                            S.barrier()
                            return

                        def vidx(rho, n, nh=nh):
                            return rho * (nh + 1) + (n - (nh - 1))
                        vtiles = []
                        for rho in range(d):
                            for n in range(nh - 1, nb):
                                start = rho + d * 128 * n
                                vtiles.append((vidx(rho, n), start))
                        for i0 in range(0, len(vtiles), 8):
                            grp = vtiles[i0:i0 + 8]
                            pb, b_pb = psb_next()
                            for j, (vi, start) in enumerate(grp):
                                tsl = slice(start, start + 127 * d + 1, d)
                                vb_ = [b_vTf[x] for x in range(start // 512, (start + 127 * d) // 512 + 1)]
                                op('pe', lambda e, j=j, tsl=tsl: e.transpose(out=pb[:, j * 128:(j + 1) * 128], in_=vTf[:, tsl], identity=identb), vb_ + [b_cstb], [b_pb])
                            v0 = grp[0][0]
                            k_ = len(grp)
                            assert [g_[0] for g_ in grp] == list(range(v0, v0 + k_))
                            op('act', lambda e: e.copy(out=Vt[:, v0:v0 + k_, :, 0:64], in_=pb[:, 0:k_ * 128].rearrange("p (k h d) -> p k h d", k=k_, h=NH)),
                               [b_pb, b_Vones], [b_Vt[x] for x in range(v0, v0 + k_)])
                        if DIL_STOP <= 3:
                            S.barrier()
                            return
                        actr = 0
                        for rho in range(d):
                            for n in range(nh, nb):
                                pT, b_pT = pTr[actr % 3]
                                actr += 1
                                t0 = rho + d * 128 * (n - nh)
                                qsl = slice(t0, t0 + 127 * d + 1, d)
                                qb_ = [b_qTf[x] for x in range(t0 // 512, (t0 + 127 * d) // 512 + 1)]
                                pSh = [ps_next(), ps_next()]
                                for kt in range(2):
                                    ks = rho + d * 128 * (n - 1 + kt)
                                    ksl = slice(ks, ks + 127 * d + 1, d)
                                    kb_ = [b_kTf[x] for x in range(ks // 512, (ks + 127 * d) // 512 + 1)]
                                    for h in range(NH):
                                        op('pe', lambda e, kt=kt, h=h, ksl=ksl: e.matmul(pSh[h][0][:, kt * 128:(kt + 1) * 128], lhsT=kTf[h * 64:(h + 1) * 64, ksl],
                                                                                       rhs=qTf[h * 64:(h + 1) * 64, qsl], start=True, stop=True),
                                           kb_ + qb_, [pSh[h][1]])
                                for h in range(NH):
                                    op('act', lambda e, h=h: e.activation(out=pT[:, 2 * h:2 * h + 2, :].rearrange("p a q -> p (a q)"), in_=pSh[h][0][:, 0:256], func=AF.Exp, scale=0.125),
                                       [pSh[h][1]], [b_pT])
                                if DIL_STOP == 41:
                                    continue
                                op('pool' if actr % 2 else 'dve', lambda e: e.tensor_tensor(out=pT[:], in0=pT[:], in1=maskc[:], op=ALU.mult), [b_pT, b_maskc], [b_pT])
                                if n == nh:
                                    for h in range(NH):
                                        op('dve', lambda e, h=h: e.tensor_scalar(out=pT[:, 2 * h, :], in0=pT[:, 2 * h, :], scalar1=flag[:, 0:1], scalar2=None, op0=ALU.mult),
                                           [b_pT, b_flag], [b_pT])
                                if DIL_STOP == 42:
                                    continue
                                pO, b_pO = ps_next()
                                for h in range(NH):
                                    for kt in range(2):
                                        vi = vidx(rho, n - 1 + kt)
                                        op('pe', lambda e, h=h, kt=kt, vi=vi: e.matmul(pO[0:65, h * 128:(h + 1) * 128], lhsT=Vt[:, vi, h, :], rhs=pT[:, h * 2 + kt, :],
                                                                                      start=(kt == 0), stop=(kt == 1)),
                                           [b_Vt[vi], b_pT], [b_pO])
                                if DIL_STOP == 43:
                                    continue
                                av = acc[:, :, qsl]
                                pv_ = pO[0:65, 0:NH * 128].rearrange("p (h q) -> p h q", h=NH)
                                if g == 0:
                                    op('dve', lambda e: e.tensor_copy(out=av, in_=pv_), [b_pO], [b_acc])
                                else:
                                    op('dve', lambda e: e.tensor_tensor(out=av, in0=av, in1=pv_, op=ALU.add), [b_pO, b_acc], [b_acc])
                        if DIL_STOP <= 4 or DIL_STOP in (41, 42, 43):
                            S.barrier()
                            return
                    if DIL_STOP <= 5:
                        S.barrier()
                        return
                    for sl in range(4):
                        ts_ = slice(sl * 512, (sl + 1) * 512)
                        pNn, b_pNn = ps_next()
                        pDn, b_pDn = ps_next()
                        for h in range(NH):
                            sh = K_SH0 if h == 0 else K_SH1
                            op('pe', lambda e, h=h, sh=sh: e.matmul(pNn[:], lhsT=cst[0:64, sh:sh + 128], rhs=acc[0:64, h, ts_], start=(h == 0), stop=(h == 1)), [b_acc, b_cst], [b_pNn])
                        for h in range(NH):
                            op('pe', lambda e, h=h: e.matmul(pDn[:], lhsT=cst[64:65, K_BM:K_BM + 128] if h == 1 else cst[64:65, K_LO:K_LO + 128], rhs=acc[64:65, h, ts_],
                                                             start=(h == 0), stop=(h == 1)), [b_acc, b_cst], [b_pDn])
                        op('dve', lambda e: e.reciprocal(out=rec[:], in_=pDn[:]), [b_pDn], [b_rec])
                        op('dve', lambda e: e.tensor_tensor(out=ydT[:, hp, ts_], in0=pNn[:], in1=rec[:], op=ALU.mult), [b_pNn, b_rec], [b_ydT])
                S.barrier()
            if 'ydT' in dbg_out:
                tmpd, b_tmpd = sb('dbgy', [128, 2 * TOK])
                op('dve', lambda e: e.tensor_copy(out=tmpd[:, 0:TOK], in_=ydT[:, 0, :]), [b_ydT], [b_tmpd])
                op('dve', lambda e: e.tensor_copy(out=tmpd[:, TOK:], in_=ydT[:, 2, :]), [b_ydT, b_tmpd], [b_tmpd])
                dump('ydT', tmpd[:], b_tmpd)
            wud, _ = sb('wud', [128, 4, 1024], BF16)
            bwud = load_w(wud, w_up_dil, 4, 1024, name='wud')
            wg1, _ = sb('wg1', [128, 8, 1024], BF16)
            bwg1 = load_w(wg1, w_in[:, C_G + 1024:C_G + 2048], 8, 1024, name='wg1')
            sg, b_sg = sb('sg4', [128, 1024])
            tmpm, b_tmpm = sb('tmpm4', [128, 1024])
            mtile, b_mtile = None, None
            for t in range(NT):
                pU = []
                b_pU = []
                for nb_ in range(2):
                    p, b_p = ps_next()
                    for hp in range(4):
                        op('pe', lambda e, p=p, hp=hp, nb_=nb_, t=t: e.matmul(p[:], lhsT=ydT[:, hp, t * 128:(t + 1) * 128], rhs=wud[:, hp, nb_ * 512:(nb_ + 1) * 512],
                                                                          start=(hp == 0), stop=(hp == 3)), [b_ydT] + bwud, [b_p])
                    pU.append(p)
                    b_pU.append(b_p)
                gate_and_merge(t, pU, b_pU, wg1, bwg1, True, sg, b_sg, tmpm, b_tmpm, mtile, b_mtile)
            S.barrier()

    if upto >= 1:
        dts.close()
    if upto >= 4:
        phase_dil()
    ctxs.close()
    if upto >= 3:
        phase_ssd_final()

    if upto >= 5:
        with ExitStack() as ph:
            sb = mk_sb(ph)
            memT, b_memT = sb('memT', [128, 8, 256], BF16)
            kTm, b_kTm = sb('kTm', [128, 12, 256], BF16)
            vtok, b_vtok = sb('vtok', [128, 2, 1536], BF16)
            gqm, b_gqm = bcast_load(sb, 'gqm', mem_q_norm, 384)
            gkm, b_gkm = bcast_load(sb, 'gkm', mem_k_norm, 384)
            qf, b_qf = sb('qf', [128, 1536])
            sq, b_sq = sb('sq5', [128, 1536])
            qb, b_qb = sb('qb', [128, 1536], BF16)
            st, b_st = sb('st5', [128, 8])
            with ExitStack() as ph2:
                sb2 = mk_sb(ph2)
                gmem, b_gmem = bcast_load(sb2, 'gmem', mem_norm, 1024)
                xm, b_xm = sb2('xm', [128, 1024])
                hm, b_hm = sb2('hm', [128, 1024], BF16)
                wkv, _ = sb2('wkv', [128, 8, 512], BF16)
                ktok, b_ktok = sb2('ktok', [128, 2, 1536])
                for mt in range(2):
                    S.dma('sp', xm[:], mem_d[mt * 128:(mt + 1) * 128, :], [], [b_xm], b_xm)
                    op('act', lambda e: e.activation(out=sq[:, 0:1024], in_=xm[:], func=AF.Square, accum_out=st[:, 0:1]), [b_xm], [b_sq, b_st])
                    rstd_from_ssq(st[:, 0:1], st[:, 1:2], 1024, b_st)
                    op('dve', lambda e: e.scalar_tensor_tensor(out=hm[:], in0=xm[:], scalar=st[:, 1:2], in1=gmem[:], op0=ALU.mult, op1=ALU.mult),
                       [b_xm, b_st, b_gmem], [b_hm])
                    pb, b_pb = psb_next()
                    for kc in range(8):
                        op('pe', lambda e, kc=kc, pb=pb: e.transpose(out=pb[:, kc * 128:(kc + 1) * 128], in_=hm[:, kc * 128:(kc + 1) * 128], identity=identb),
                           [b_hm, b_cstb], [b_pb])
                    op('act', lambda e, mt=mt, pb=pb: e.copy(out=memT[:, :, mt * 128:(mt + 1) * 128], in_=pb[:].rearrange("p (k t) -> p k t", k=8)), [b_pb], [b_memT])
                for nb in range(6):
                    bw = load_w(wkv, w_mem_kv[:, nb * 512:(nb + 1) * 512], 8, 512, name='wkv')
                    for mt in range(2):
                        p, b_p = ps_next()
                        for kc in range(8):
                            op('pe', lambda e, p=p, kc=kc, mt=mt: e.matmul(p[:], lhsT=memT[:, kc, mt * 128:(mt + 1) * 128], rhs=wkv[:, kc, :], start=(kc == 0), stop=(kc == 7)),
                               [b_memT] + bw, [b_p])
                        if nb < 3:
                            op('act', lambda e, p=p, mt=mt, nb=nb: e.copy(out=ktok[:, mt, nb * 512:(nb + 1) * 512], in_=p[:]), [b_p], [b_ktok])
                        else:
                            op('act', lambda e, p=p, mt=mt, nb=nb: e.copy(out=vtok[:, mt, (nb - 3) * 512:(nb - 2) * 512], in_=p[:]), [b_p], [b_vtok])
                for mt in range(2):
                    op('act', lambda e, mt=mt: e.activation(out=sq[:], in_=ktok[:, mt, :], func=AF.Square), [b_ktok], [b_sq])
                    op('dve', lambda e: e.tensor_reduce(out=st[:, 0:4], in_=sq[:].rearrange("p (h d) -> p h d", h=4), axis=AX.X, op=ALU.add), [b_sq], [b_st])
                    rstd_from_ssq(st[:, 0:4], st[:, 4:8], 384, b_st)
                    op('dve', lambda e, mt=mt: e.tensor_tensor(out=qf[:].rearrange("p (h d) -> p h d", h=4), in0=ktok[:, mt, :].rearrange("p (h d) -> p h d", h=4),
                                                               in1=st[:, 4:8].unsqueeze(2).to_broadcast([128, 4, 384]), op=ALU.mult), [b_ktok, b_st], [b_qf])
                    op('pool', lambda e: e.tensor_tensor(out=qb[:].rearrange("p (h d) -> p h d", h=4), in0=qf[:].rearrange("p (h d) -> p h d", h=4),
                                                         in1=gkm[:].unsqueeze(1).to_broadcast([128, 4, 384]), op=ALU.mult), [b_qf, b_gkm], [b_qb])
                    for half, cnt in ((0, 8), (1, 4)):
                        pb, b_pb = psb_next()
                        for k in range(cnt):
                            kc = half * 8 + k
                            op('pe', lambda e, pb=pb, k=k, kc=kc: e.transpose(out=pb[:, k * 128:(k + 1) * 128], in_=qb[:, kc * 128:(kc + 1) * 128], identity=identb),
                               [b_qb, b_cstb], [b_pb])
                        op('act', lambda e, pb=pb, half=half, cnt=cnt, mt=mt: e.copy(out=kTm[:, half * 8:half * 8 + cnt, mt * 128:(mt + 1) * 128],
                                                                                 in_=pb[:, 0:cnt * 128].rearrange("p (k t) -> p k t", k=cnt)), [b_pb], [b_kTm])
                S.barrier()
            wqm, _ = sb('wqm', [128, 8, 1536], BF16)
            bwqm = load_w(wqm, w_in[:, C_QM:C_QM + 1536], 8, 1536, name='wqm')
            wum, _ = sb('wum', [128, 12, 1024], BF16)
            bwum = load_w(wum, w_up_mem, 12, 1024, name='wum')
            wg2, _ = sb('wg2', [128, 8, 1024], BF16)
            bwg2 = load_w(wg2, w_in[:, C_G + 2048:C_G + 3072], 8, 1024, name='wg2')
            qmT, b_qmT = sb('qmT', [128, 12, 128], BF16)
            pTm = [sb('pTm%d' % i, [128, 512], BF16) for i in range(2)]
            recm, b_recm = sb('recm', [128, 128])
            ymT, b_ymT = sb('ymT', [128, 12, 128], BF16)
            sg, b_sg = sb('sg5', [128, 1024])
            tmpm, b_tmpm = sb('tmpm5', [128, 1024])
            mtile, b_mtile = sb('mtile5', [128, 1024])
            onesb = cstb[:, K_ONES:K_ONES + 128]
            for t in range(NT):
                for nb in range(3):
                    p, b_p = ps_next()
                    for kc in range(8):
                        op('pe', lambda e, p=p, kc=kc, nb=nb, t=t: e.matmul(p[:], lhsT=hsl(kc, own(t)), rhs=wqm[:, kc, nb * 512:(nb + 1) * 512], start=(kc == 0), stop=(kc == 7)),
                           [b_hT[16 + t]] + bwqm, [b_p])
                    op('act', lambda e, p=p, nb=nb: e.copy(out=qf[:, nb * 512:(nb + 1) * 512], in_=p[:]), [b_p], [b_qf])
                op('act', lambda e: e.activation(out=sq[:], in_=qf[:], func=AF.Square), [b_qf], [b_sq])
                op('dve', lambda e: e.tensor_reduce(out=st[:, 0:4], in_=sq[:].rearrange("p (h d) -> p h d", h=4), axis=AX.X, op=ALU.add), [b_sq], [b_st])
                rstd_from_ssq(st[:, 0:4], st[:, 4:8], 384, b_st)
                op('dve', lambda e: e.tensor_tensor(out=qf[:].rearrange("p (h d) -> p h d", h=4), in0=qf[:].rearrange("p (h d) -> p h d", h=4),
                                                    in1=st[:, 4:8].unsqueeze(2).to_broadcast([128, 4, 384]), op=ALU.mult), [b_qf, b_st], [b_qf])
                op('pool', lambda e: e.tensor_tensor(out=qb[:].rearrange("p (h d) -> p h d", h=4), in0=qf[:].rearrange("p (h d) -> p h d", h=4),
                                                     in1=gqm[:].unsqueeze(1).to_broadcast([128, 4, 384]), op=ALU.mult), [b_qf, b_gqm], [b_qb])
                for half, cnt in ((0, 8), (1, 4)):
                    pb, b_pb = psb_next()
                    for k in range(cnt):
                        kc = half * 8 + k
                        op('pe', lambda e, pb=pb, k=k, kc=kc: e.transpose(out=pb[:, k * 128:(k + 1) * 128], in_=qb[:, kc * 128:(kc + 1) * 128], identity=identb),
                           [b_qb, b_cstb], [b_pb])
                    op('act', lambda e, pb=pb, half=half, cnt=cnt: e.copy(out=qmT[:, half * 8:half * 8 + cnt, :], in_=pb[:, 0:cnt * 128].rearrange("p (k t) -> p k t", k=cnt)),
                       [b_pb], [b_qmT])
                for mt in range(2):
                    pS, b_pS = ps_next()
                    for h in range(4):
                        for j in range(3):
                            op('pe', lambda e, pS=pS, h=h, j=j, mt=mt: e.matmul(pS[:, h * 128:(h + 1) * 128], lhsT=kTm[:, h * 3 + j, mt * 128:(mt + 1) * 128], rhs=qmT[:, h * 3 + j, :],
                                                                             start=(j == 0), stop=(j == 2)), [b_kTm, b_qmT], [b_pS])
                    op('act', lambda e, pS=pS, mt=mt: e.activation(out=pTm[mt][0][:], in_=pS[:], func=AF.Exp, scale=1.0 / math.sqrt(384.0)), [b_pS], [pTm[mt][1]])
                for h in range(4):
                    pN, b_pN = ps_next()
                    for j in range(4):
                        for mt in range(2):
                            lhs = vtok[:, mt, h * 384 + j * 128:h * 384 + (j + 1) * 128] if j < 3 else onesb
                            op('pe', lambda e, pN=pN, j=j, mt=mt, h=h, lhs=lhs: e.matmul(pN[:, j * 128:(j + 1) * 128], lhsT=lhs, rhs=pTm[mt][0][:, h * 128:(h + 1) * 128],
                                                                                      start=(mt == 0), stop=(mt == 1)), [b_vtok, b_cstb, pTm[mt][1]], [b_pN])
                    op('dve', lambda e, pN=pN: e.reciprocal(out=recm[:], in_=pN[:, 384:512]), [b_pN], [b_recm])
                    op('dve', lambda e, pN=pN, h=h: e.tensor_tensor(out=ymT[:, h * 3:(h + 1) * 3, :], in0=pN[:, 0:384].rearrange("p (j q) -> p j q", j=3),
                                                                    in1=recm[:].unsqueeze(1).to_broadcast([128, 3, 128]), op=ALU.mult), [b_pN, b_recm], [b_ymT])
                if t == 0 and 'ymT' in dbg_out:
                    tmpd, b_tmpd = sb('dbgm', [128, 1536])
                    op('dve', lambda e: e.tensor_copy(out=tmpd[:], in_=ymT[:].rearrange("p k t -> p (k t)")), [b_ymT], [b_tmpd])
                    dump('ymT', tmpd[:], b_tmpd)
                pU = []
                b_pU = []
                for nb in range(2):
                    p, b_p = ps_next()
                    for kc in range(12):
                        op('pe', lambda e, p=p, kc=kc, nb=nb: e.matmul(p[:], lhsT=ymT[:, kc, :], rhs=wum[:, kc, nb * 512:(nb + 1) * 512], start=(kc == 0), stop=(kc == 11)),
                           [b_ymT] + bwum, [b_p])
                    pU.append(p)
                    b_pU.append(b_p)
                gate_and_merge(t, pU, b_pU, wg2, bwg2, False, sg, b_sg, tmpm, b_tmpm, mtile, b_mtile)
            S.barrier()
    S.barrier()
    mid.close()

    if upto >= 6:
        late = ExitStack()
        sbl = mk_sb(late)
        rstd2, b_rstd2 = sbl('rstd2', [128, NT])
        gffn, b_gffn = bcast_load(sbl, 'gffn', norm_ffn, 1024)
        eidx, _ = sbl('eidx', [128, NT, 128], U32)
        gate, _ = sbl('gate', [128, NT, 128])
        b_eidx = [Buf('eidx%d' % t) for t in range(NT)]
        b_gate = [Buf('gate%d' % t) for t in range(NT)]
        keysT, b_keysT = sbl('keysT', [128, 256])
        qsc = nc.dram_tensor('qsc', [8, 128, 4096], F32, kind="Internal").ap()
        b_qsc = [Buf('qsc%d' % i) for i in range(8)]
        with ExitStack() as ph:
            sb = mk_sb(ph)
            h2T, _ = sb('h2T', [128, 8, TOK], BF16)
            b_h2T = [Buf('h2T%d' % t) for t in range(NT)]
            with ExitStack() as pha:
                sb = mk_sb(pha)
                wo, _ = sb('wo', [128, 8, 1024], BF16)
                bwo = load_w(wo, w_out, 8, 1024, name='wo')
                mbr = [sb('mb%d' % i, [128, 1024], BF16) for i in range(2)]
                mTr = [sb('mT%d' % i, [128, 8, 128], BF16) for i in range(2)]
                xts = [sb('xo%d' % i, [128, 1024]) for i in range(3)]
                mts = [sb('mo%d' % i, [128, 1024]) for i in range(2)]
                x1s = [sb('x1o%d' % i, [128, 1024]) for i in range(2)]
                sq, b_sq = sb('sq6', [128, 1024])
                str6 = [sb('st6_%d' % i, [128, 2]) for i in range(2)]
                hbr6 = [sb('hb6_%d' % i, [128, 1024], BF16) for i in range(2)]
                live = {}

                def stA(t):
                    xt, b_xt = xts[t % 3]
                    mtl, b_mtl = mts[t % 2]
                    mb_, b_mb = mbr[t % 2]
                    S.dma('sp', xt[:], xa[TOK + t * 128:TOK + (t + 1) * 128, :], [], [b_xt], b_xt)
                    S.dma('sp', mtl[:], msc[t * 128:(t + 1) * 128, :], [b_msc[t]], [b_mtl], b_mtl)
                    op('dve', lambda e: e.tensor_copy(out=mb_[:], in_=mtl[:]), [b_mtl], [b_mb])
                    pb, b_pb = psb_next()
                    for kc in range(8):
                        op('pe', lambda e, kc=kc: e.transpose(out=pb[:, kc * 128:(kc + 1) * 128], in_=mb_[:, kc * 128:(kc + 1) * 128], identity=identb), [b_mb, b_cstb], [b_pb])
                    live[('A', t)] = (pb, b_pb)

                def stB(t):
                    pb, b_pb = live.pop(('A', t))
                    mT, b_mT = mTr[t % 2]
                    op('act', lambda e: e.copy(out=mT[:], in_=pb[:].rearrange("p (k t) -> p k t", k=8)), [b_pb], [b_mT])
                    ps_ = []
                    for nb in range(2):
                        p, b_p = ps_next()
                        for kc in range(8):
                            op('pe', lambda e, kc=kc, nb=nb: e.matmul(p[:], lhsT=mT[:, kc, :], rhs=wo[:, kc, nb * 512:(nb + 1) * 512], start=(kc == 0), stop=(kc == 7)),
                               [b_mT] + bwo, [b_p])
                        ps_.append((p, b_p))
                    live[('B', t)] = ps_

                def stC(t):
                    ps_ = live.pop(('B', t))
                    xt, b_xt = xts[t % 3]
                    x1t, b_x1t = x1s[t % 2]
                    st, b_st = str6[t % 2]
                    hb, b_hb = hbr6[t % 2]
                    for nb in range(2):
                        p, b_p = ps_[nb]
                        op('dve', lambda e, nb=nb: e.tensor_tensor(out=x1t[:, nb * 512:(nb + 1) * 512], in0=p[:], in1=xt[:, nb * 512:(nb + 1) * 512], op=ALU.add),
                           [b_p, b_xt], [b_x1t])
                    S.dma('sp', out_d[t * 128:(t + 1) * 128, :], x1t[:], [b_x1t], [b_x1d[t]], b_x1t)
                    if t == 0:
                        dump('x1', x1t[:], b_x1t)
                    op('act', lambda e: e.activation(out=sq[:], in_=x1t[:], func=AF.Square, accum_out=st[:, 0:1]), [b_x1t], [b_sq, b_st])
                    op('act', lambda e: e.activation(out=rstd2[:, t:t + 1], in_=st[:, 0:1], func=AF.Sqrt, scale=1.0 / 1024, bias=epst[:, 0:1]), [b_st, b_eps], [b_rstd2])
                    op('dve', lambda e: e.reciprocal(out=rstd2[:, t:t + 1], in_=rstd2[:, t:t + 1]), [b_rstd2], [b_rstd2])
                    op('dve', lambda e: e.scalar_tensor_tensor(out=hb[:], in0=x1t[:], scalar=rstd2[:, t:t + 1], in1=gffn[:], op0=ALU.mult, op1=ALU.mult),
                       [b_x1t, b_rstd2, b_gffn], [b_hb])
                    pb, b_pb = psb_next()
                    for kc in range(8):
                        op('pe', lambda e, kc=kc: e.transpose(out=pb[:, kc * 128:(kc + 1) * 128], in_=hb[:, kc * 128:(kc + 1) * 128], identity=identb), [b_hb, b_cstb], [b_pb])
                    live[('C', t)] = (pb, b_pb)

                def stD(t):
                    pb, b_pb = live.pop(('C', t))
                    op('act', lambda e: e.copy(out=h2T[:, :, t * 128:(t + 1) * 128], in_=pb[:].rearrange("p (k t) -> p k t", k=8)), [b_pb], [b_h2T[t]])
                stages = (stA, stB, stC, stD)
                for step in range(NT + 3):
                    for j in (3, 2, 1, 0):
                        t = step - j
                        if 0 <= t < NT:
                            stages[j](t)
                S.barrier()
            sb = mk_sb(ph)
            wpq, _ = sb('wpq', [128, 8, 2048], BF16)
            bwpq = load_w(wpq, peer_w_q, 8, 2048, name='wpq')
            kr_, b_kr_ = sb('keysr', [128, 256])
            S.dma('sp', kr_[:, 0:128], peer_keys1, [], [b_kr_], b_kr_)
            S.dma('sp', kr_[:, 128:256], peer_keys2, [b_kr_], [b_kr_], b_kr_)
            p, b_p = ps_next()
            for i in range(2):
                op('pe', lambda e, p=p, i=i: e.transpose(out=p[:, i * 128:(i + 1) * 128], in_=kr_[:, i * 128:(i + 1) * 128], identity=identf), [b_kr_, b_cst], [b_p])
            op('act', lambda e, p=p: e.copy(out=keysT[:], in_=p[:, 0:256]), [b_p], [b_keysT])
            qTsr = [sb('qTs%d' % i, [128, 16, 256]) for i in range(2)]
            for sl in range(8):
                qTs, b_qTs = qTsr[sl % 2]
                for c in range(16):
                    p, b_p = ps_next()
                    for kc in range(8):
                        op('pe', lambda e, p=p, kc=kc, c=c, sl=sl: e.matmul(p[:, 0:256], lhsT=wpq[:, kc, c * 128:(c + 1) * 128], rhs=h2T[:, kc, sl * 256:(sl + 1) * 256],
                                                                         start=(kc == 0), stop=(kc == 7)), b_h2T[2 * sl:2 * sl + 2] + bwpq, [b_p])
                    op('act', lambda e, p=p, c=c, qTs=qTs: e.copy(out=qTs[:, c, :], in_=p[:, 0:256]), [b_p], [b_qTs])
                S.dma('sp', qsc[sl], qTs[:].rearrange("p c k -> p (c k)"), [b_qTs], [b_qsc[sl]], b_qTs)
            S.barrier()

        if upto >= 7:
            with ExitStack() as ph:
                sb = mk_sb(ph)
                NU = 16
                GS = 8
                uvs = [sb('uv%d' % i, [128, 2048], BF16) for i in range(NU)]
                dgs = [sb('dg%d' % i, [128, 128], BF16) for i in range(4)]
                h2f, b_h2f = sb('h2f', [128, 1024], BF16)
                junkr = [sb('junk%d' % i, [128, 1024], BF16) for i in range(4)]
                dotr = [sb('dots%d' % i, [128, GS]) for i in range(2)]
                wgr = [sb('wgt%d' % i, [128, GS]) for i in range(2)]
                ots = [sb('ot%d' % i, [128, 1024]) for i in range(2)]
                x1p = [sb('x1p%d' % i, [128, 1024]) for i in range(2)]
                qTtr = [sb('qTt%d' % i, [128, 16, 128]) for i in range(2)]
                scs, b_scs = sb('scs', [128, 16, 128])
                sc2, b_sc2 = sb('sc2', [128, 16, 128])
                vals, b_vals = sb('vals', [128, 16, 16])
                idxs, b_idxs = sb('idxs', [128, 16, 16], U32)
                idxf, b_idxf = sb('idxf', [128, 16, 16])
                cand, b_cand = sb('cand', [128, 8, 256])
                cand2, b_cand2 = sb('cand2', [128, 8, 256])
                scv, b_scv = sb('scv', [128, 8, 16])
                ci, b_ci = sb('ci', [128, 8, 16], U32)
                cia, b_cia = sb('cia', [128, 8, 16], U32)
                cib, b_cib = sb('cib', [128, 8, 16], U32)
                fa, b_fa = sb('fa', [128, 8, 16])
                fb, b_fb = sb('fb', [128, 8, 16])
                eq, b_eq = sb('eq', [128, 8, 16, 16])
                e1, b_e1 = sb('e1', [128, 8, 16])
                e2, b_e2 = sb('e2', [128, 8, 16])
                nm, b_nm = sb('nm', [128, 8])
                sm, b_sm = sb('sm', [128, 8])
                ex, b_ex = sb('ex', [128, 8, 16])
                iota16 = cst[:, K_IOTA:K_IOTA + 16]

                def route_gen(t):
                    qTt, b_qTt = qTtr[t % 2]
                    S.dma('sp', qTt[:], qsc[t // 2].rearrange("p (c k) -> p c k", c=16)[:, :, (t % 2) * 128:(t % 2 + 1) * 128], [b_qsc[t // 2]], [b_qTt], b_qTt)
                    yield
                    for q4 in range(4):
                        p, b_p = ps_next()
                        for cc in range(4):
                            c = q4 * 4 + cc
                            op('pe', lambda e, p=p, c=c, cc=cc: e.matmul(p[:, cc * 128:(cc + 1) * 128], lhsT=qTt[:, c, :],
                                                                       rhs=keysT[:, (c % 2) * 128:(c % 2 + 1) * 128], start=True, stop=True), [b_qTt, b_keysT], [b_p])
                        op('act', lambda e, p=p, q4=q4: e.copy(out=scs[:, q4 * 4:(q4 + 1) * 4, :], in_=p[:].rearrange("p (c k) -> p c k", c=4)), [b_p], [b_scs])
                        yield
                    bv = [Buf('vals%d' % c) for c in range(16)]
                    bi = [Buf('idxs%d' % c) for c in range(16)]
                    b2 = [Buf('sc2_%d' % c) for c in range(16)]
                    for c in range(16):
                        op('dve', lambda e, c=c: e.max(out=vals[:, c, 0:8], in_=scs[:, c, :]), [b_scs, b_vals], [bv[c]])
                        if c % 4 == 3:
                            yield
                    for c in range(16):
                        op('dve', lambda e, c=c: e.max_index(out=idxs[:, c, 0:8], in_max=vals[:, c, 0:8], in_values=scs[:, c, :]), [b_scs, bv[c], b_idxs], [bi[c]])
                        if c % 4 == 3:
                            yield
                    for c in range(16):
                        op('dve', lambda e, c=c: e.match_replace(out=sc2[:, c, :], in_to_replace=vals[:, c, 0:8], in_values=scs[:, c, :], imm_value=-1e30), [b_scs, bv[c], b_sc2], [b2[c]])
                        if c % 4 == 3:
                            yield
                    for c in range(16):
                        op('dve', lambda e, c=c: e.max(out=vals[:, c, 8:16], in_=sc2[:, c, :]), [b2[c]], [bv[c]])
                        if c % 4 == 3:
                            yield
                    for c in range(16):
                        op('dve', lambda e, c=c: e.max_index(out=idxs[:, c, 8:16], in_max=vals[:, c, 8:16], in_values=sc2[:, c, :]), [b2[c], bv[c]], [bi[c]])
                        if c % 4 == 3:
                            yield
                    op('dve', lambda e: e.tensor_copy(out=idxf[:], in_=idxs[:]), bi, [b_idxf, b_idxs])
                    v4 = vals[:].rearrange("p (h two) k -> p h two k", two=2)
                    op('dve', lambda e, v4=v4: e.tensor_tensor(out=cand[:].rearrange("p h (a b) -> p h a b", a=16), in0=v4[:, :, 0, :].unsqueeze(3).to_broadcast([128, 8, 16, 16]),
                                                               in1=v4[:, :, 1, :].unsqueeze(2).to_broadcast([128, 8, 16, 16]), op=ALU.add), bv + b2, [b_cand, b_vals, b_sc2])
                    bs_ = [Buf('scv%d' % h) for h in range(8)]
                    bc_ = [Buf('ci%d' % h) for h in range(8)]
                    b3 = [Buf('cand2_%d' % h) for h in range(8)]
                    for h in range(8):
                        op('dve', lambda e, h=h: e.max(out=scv[:, h, 0:8], in_=cand[:, h, :]), [b_cand, b_scv], [bs_[h]])
                        if h % 4 == 3:
                            yield
                    for h in range(8):
                        op('dve', lambda e, h=h: e.max_index(out=ci[:, h, 0:8], in_max=scv[:, h, 0:8], in_values=cand[:, h, :]), [b_cand, bs_[h], b_ci], [bc_[h]])
                        if h % 4 == 3:
                            yield
                    for h in range(8):
                        op('dve', lambda e, h=h: e.match_replace(out=cand2[:, h, :], in_to_replace=scv[:, h, 0:8], in_values=cand[:, h, :], imm_value=-1e30), [b_cand, bs_[h], b_cand2], [b3[h]])
                        if h % 4 == 3:
                            yield
                    for h in range(8):
                        op('dve', lambda e, h=h: e.max(out=scv[:, h, 8:16], in_=cand2[:, h, :]), [b3[h]], [bs_[h]])
                        if h % 4 == 3:
                            yield
                    for h in range(8):
                        op('dve', lambda e, h=h: e.max_index(out=ci[:, h, 8:16], in_max=scv[:, h, 8:16], in_values=cand2[:, h, :]), [b3[h], bs_[h]], [bc_[h]])
                        if h % 4 == 3:
                            yield
                    yield
                    op('dve', lambda e: e.tensor_copy(out=cand2[:, 0, 0:1], in_=cand2[:, 0, 0:1]), bs_ + bc_ + b3, [b_scv, b_ci, b_cand2])
                    op('dve', lambda e: e.tensor_scalar(out=cia[:], in0=ci[:], scalar1=4, scalar2=None, op0=ALU.arith_shift_right), [b_ci], [b_cia])
                    op('dve', lambda e: e.tensor_scalar(out=cib[:], in0=ci[:], scalar1=15, scalar2=None, op0=ALU.bitwise_and), [b_ci], [b_cib])
                    op('dve', lambda e: e.tensor_copy(out=fa[:], in_=cia[:]), [b_cia], [b_fa])
                    op('dve', lambda e: e.tensor_copy(out=fb[:], in_=cib[:]), [b_cib], [b_fb])
                    yield
                    i4 = idxf[:].rearrange("p (h two) k -> p h two k", two=2)
                    iob = iota16.unsqueeze(1).unsqueeze(1).to_broadcast([128, 8, 16, 16])
                    for (fsel, which, eo, b_eo) in ((fa, 0, e1, b_e1), (fb, 1, e2, b_e2)):
                        fbuf = b_fa if which == 0 else b_fb
                        op('dve', lambda e, fsel=fsel: e.tensor_tensor(out=eq[:], in0=fsel[:].unsqueeze(3).to_broadcast([128, 8, 16, 16]), in1=iob, op=ALU.is_equal),
                           [fbuf, b_cst], [b_eq])
                        op('dve', lambda e, which=which, i4=i4: e.tensor_tensor(out=eq[:], in0=eq[:], in1=i4[:, :, which, :].unsqueeze(2).to_broadcast([128, 8, 16, 16]), op=ALU.mult),
                           [b_eq, b_idxf], [b_eq])
                        op('dve', lambda e, eo=eo: e.tensor_reduce(out=eo[:], in_=eq[:], axis=AX.X, op=ALU.add), [b_eq], [b_eo])
                        yield
                    op('dve', lambda e: e.scalar_tensor_tensor(out=e1[:], in0=e1[:], scalar=128.0, in1=e2[:], op0=ALU.mult, op1=ALU.add), [b_e1, b_e2], [b_e1])
                    op('dve', lambda e, t=t: e.tensor_copy(out=eidx[:, t, :], in_=e1[:].rearrange("p h k -> p (h k)")), [b_e1], [b_eidx[t]])
                    op('dve', lambda e: e.tensor_scalar(out=nm[:], in0=scv[:, :, 0], scalar1=-1.0, scalar2=None, op0=ALU.mult), [b_scv], [b_nm])
                    for h in range(8):
                        op('act', lambda e, h=h: e.activation(out=ex[:, h, :], in_=scv[:, h, :], func=AF.Exp, bias=nm[:, h:h + 1], accum_out=sm[:, h:h + 1]),
                           [b_scv, b_nm], [b_ex, b_sm])
                        if h % 4 == 3:
                            yield
                    op('dve', lambda e: e.reciprocal(out=sm[:], in_=sm[:]), [b_sm], [b_sm])
                    op('dve', lambda e, t=t: e.tensor_tensor(out=gate[:, t, :].rearrange("p (h k) -> p h k", h=8), in0=ex[:], in1=sm[:].unsqueeze(2).to_broadcast([128, 8, 16]), op=ALU.mult),
                       [b_ex, b_sm], [b_gate[t]])
                    yield

                rr['n'] = 4
                for _ in route_gen(0):
                    pass
                for t in range(NT):
                    rg = route_gen(t + 1) if t + 1 < NT else iter(())
                    x1t, b_x1t = x1p[t % 2]
                    S.dma('sp', x1t[:], out_d[t * 128:(t + 1) * 128, :], [b_x1d[t]], [b_x1t], b_x1t)
                    op('dve', lambda e, t=t, x1t=x1t: e.scalar_tensor_tensor(out=h2f[:], in0=x1t[:], scalar=rstd2[:, t:t + 1], in1=gffn[:], op0=ALU.mult, op1=ALU.mult),
                       [b_x1t, b_rstd2, b_gffn], [b_h2f])
                    pA = [psf[4], psf[5]]
                    for sg_ in range(128 // GS):
                        dots, b_dots = dotr[sg_ % 2]
                        wgt, b_wgt = wgr[sg_ % 2]
                        for k in range(GS):
                            s = sg_ * GS + k
                            uv, b_uv = uvs[s % NU]
                            S.dma('pool', None, None, [b_eidx[t]], [b_uv], b_uv,
                                  indirect=lambda e, uv=uv, t=t, s=s: e.indirect_dma_start(out=uv[:], out_offset=None, in_=cb16,
                                                                                       in_offset=bass.IndirectOffsetOnAxis(ap=eidx[:, t, s:s + 1], axis=0)))
                            junk, b_junk = junkr[s % 4]
                            op('dve', lambda e, uv=uv, k=k, dots=dots, junk=junk: e.scalar_tensor_tensor(out=junk[:], in0=uv[:, 0:1024], scalar=1.0, in1=h2f[:], op0=ALU.mult, op1=ALU.mult,
                                                                                            accum_out=dots[:, k:k + 1]),
                               [b_uv, b_h2f], [b_junk] + ([b_dots] if k in (0, GS - 1) else []))
                        op('act', lambda e, dots=dots, wgt=wgt: e.activation(out=wgt[:], in_=dots[:], func=AF.Gelu), [b_dots], [b_wgt])
                        for _ in range(3):
                            next(rg, None)
                        op('dve', lambda e, t=t, wgt=wgt, sg_=sg_: e.tensor_tensor(out=wgt[:], in0=wgt[:], in1=gate[:, t, sg_ * GS:(sg_ + 1) * GS], op=ALU.mult), [b_wgt, b_gate[t]], [b_wgt])
                        for k in range(GS):
                            s = sg_ * GS + k
                            uv, b_uv = uvs[s % NU]
                            dg, b_dg = dgs[s % 4]
                            op('act', lambda e, dg=dg, k=k, wgt=wgt: e.activation(out=dg[:], in_=identf, func=AF.Copy, scale=wgt[:, k:k + 1]), [b_cst, b_wgt], [b_dg])
                            for nb in range(2):
                                op('pe', lambda e, nb=nb, dg=dg, uv=uv, s=s, pA=pA: e.matmul(pA[nb][0][:], lhsT=dg[:], rhs=uv[:, 1024 + nb * 512:1024 + (nb + 1) * 512],
                                                                                         start=(s == 0), stop=(s == 127)),
                                   [b_dg, b_uv], [pA[nb][1]])
                    for _ in rg:
                        pass
                    ot, b_ot = ots[t % 2]
                    for nb in range(2):
                        op('dve', lambda e, nb=nb, ot=ot, x1t=x1t, pA=pA: e.tensor_tensor(out=ot[:, nb * 512:(nb + 1) * 512], in0=pA[nb][0][:], in1=x1t[:, nb * 512:(nb + 1) * 512], op=ALU.add),
                           [pA[nb][1], b_x1t], [b_ot])
                    S.dma('sp', out_d[t * 128:(t + 1) * 128, :], ot[:], [b_ot], [b_x1d[t]], b_ot)
                rr['n'] = 6
                S.barrier()
        late.close()
    S.finish()
    top.close()
    return nc, S


_CST = None


def _consts():
    global _CST
    if _CST is None:
        c = np.zeros((128, K_END), np.float32)
        p = np.arange(128)[:, None]
        f = np.arange(128)[None, :]
        c[:, K_ID:K_ID + 128] = (p == f)
        c[:, K_TRIU:K_TRIU + 128] = (p <= f)
        c[:, K_TRIL:K_TRIL + 128] = (p >= f)
        c[:, K_TRILS:K_TRILS + 128] = (p > f)
        c[:, K_ONES:K_ONES + 128] = 1.0
        half = 32
        inv = (np.float32(10000.0) ** (-np.arange(half, dtype=np.float32) / np.float32(half))).astype(np.float32)
        c[:, K_INVF:K_INVF + 32] = inv[None, :]
        c[:, K_BM:K_BM + 128] = (p // 64 == f // 64)
        rm = np.zeros((128, 128), np.float32)
        for cc in range(128):
            if cc % 64 < 32:
                rm[cc + 32, cc] = -1.0
            else:
                rm[cc - 32, cc] = 1.0
        c[:, K_RM:K_RM + 128] = rm
        c[0:64, K_SH0:K_SH0 + 64] = np.eye(64, dtype=np.float32)
        c[0:64, K_SH1 + 64:K_SH1 + 128] = np.eye(64, dtype=np.float32)
        c[:, K_LO:K_LO + 64] = 1.0
        c[:, K_INVFC] = inv[np.arange(128) % 32]
        c[:, K_IOTA:K_IOTA + 16] = np.arange(16, dtype=np.float32)[None, :]
        _CST = c
    return _CST


def make_in_maps(inputs, cores=range(8)):
    x = np.asarray(inputs['x'], np.float32)
    pos = np.asarray(inputs['positions'], np.int32)
    mem = np.asarray(inputs['mem'], np.float32)
    shared = {}
    for k in ('norm_mix', 'conv_b', 'dt_bias', 'a_log', 'd_skip', 'ssd_norm', 'dil_q_norm', 'dil_k_norm', 'mem_norm', 'mem_q_norm', 'mem_k_norm', 'norm_ffn'):
        shared[k] = np.ascontiguousarray(np.asarray(inputs[k], np.float32).reshape(1, -1))
    for k in ('w_in', 'conv_w', 'w_mem_kv', 'w_up_ssd', 'w_up_dil', 'w_up_mem', 'w_out', 'peer_w_q', 'peer_keys1', 'peer_keys2', 'peer_u', 'peer_v'):
        shared[k] = np.ascontiguousarray(np.asarray(inputs[k], np.float32)[0])
    shared['cst'] = _consts()
    maps = []
    for c in cores:
        b, hf = c // 2, c % 2
        m = dict(shared)
        if hf == 0:
            xa = np.concatenate([np.zeros((TOK, 1024), np.float32), x[b, :TOK]], 0)
            pa = np.concatenate([np.zeros((TOK,), np.int32), pos[b, :TOK]], 0)
        else:
            xa = x[b]
            pa = pos[b]
        m['xa'] = np.ascontiguousarray(xa)
        m['posa'] = np.ascontiguousarray(pa)
        m['flag'] = np.full((128, 1), float(hf), np.float32)
        m['mem'] = np.ascontiguousarray(mem[b])
        m['gq_col'] = np.ascontiguousarray(np.tile(shared['dil_q_norm'].reshape(64), 2).reshape(128, 1))
        m['gk_col'] = np.ascontiguousarray(np.tile(shared['dil_k_norm'].reshape(64), 2).reshape(128, 1))
        maps.append(m)
    return maps


_NC = None


def kernel(**inputs):
    global _NC
    if _NC is None:
        _NC = build()[0]
    maps = make_in_maps(inputs)
    res = run_bass_kernel_spmd(_NC, maps, core_ids=list(range(8)))
    out = np.zeros((4, 4096, 1024), np.float32)
    for c in range(8):
        b, hf = c // 2, c % 2
        out[b, hf * TOK:(hf + 1) * TOK] = res.results[c]['out']
    return out
```
